# Optimizing a Trainium2 kernel written in Bass

```python
import jax
import jax.numpy as jnp
from jax import lax
import numpy as np

D_MODEL = 1024
BATCH = 2
SEQ = 8192
DEPTH = 1

NORM_EPS = 1e-6
HG_KDIM = 128
HG_VDIM = 128
HG_HEADS = (D_MODEL // 2) // HG_VDIM
HG_FDIM = HG_HEADS * HG_KDIM
HG_WIDTH = HG_HEADS * HG_VDIM
HG_CHUNK = 64
RW_HEAD = 64
RW_WIDTH = D_MODEL // 2
RW_HEADS = RW_WIDTH // RW_HEAD
RW_DECAY_LORA = 32
RW_AAA_LORA = 32
RW_GATE_LORA = 96
RW_SHIFT_COLS = 3 * RW_WIDTH + RW_DECAY_LORA + RW_AAA_LORA + RW_GATE_LORA
RW_GN_EPS = 64e-5
RW_SPLITS = (RW_WIDTH, 2 * RW_WIDTH, 3 * RW_WIDTH, 3 * RW_WIDTH + RW_DECAY_LORA,
             3 * RW_WIDTH + RW_DECAY_LORA + RW_AAA_LORA)
IN_SPLITS = (HG_FDIM, 2 * HG_FDIM, 2 * HG_FDIM + HG_WIDTH, 2 * HG_FDIM + 2 * HG_WIDTH,
             2 * HG_FDIM + 2 * HG_WIDTH + RW_SHIFT_COLS)
IN_COLS = 2 * HG_FDIM + 2 * HG_WIDTH + RW_SHIFT_COLS + 2 * D_MODEL
N_GROUPS = 4
EXPERTS_PER_GROUP = 8
N_EXPERTS = N_GROUPS * EXPERTS_PER_GROUP
TOP_K = 2
D_FF_EXPERT = 512
MOE_BLOCK = 128

kernel_name = 'hgrn2_rwkv7_gated_hier_moe_block'


def rmsnorm(x, gain, eps=NORM_EPS):
    xf = x.astype(jnp.float32)
    y = xf * lax.rsqrt(jnp.mean(xf * xf, axis=-1, keepdims=True) + eps)
    return (y * gain.astype(jnp.float32)).astype(x.dtype)


def hgrn2_mixer(q, f_raw, i, g, lb, norm_g):
    B, S, _ = q.shape
    nc = S // HG_CHUNK
    f32 = jnp.float32
    f = lb + (1.0 - lb) * jax.nn.sigmoid(f_raw.astype(f32))
    log_f = jnp.log(f)
    k = 1.0 - f
    qf = jax.nn.silu(q.astype(f32))

    def to_chunks(t, d):
        return t.reshape(B, nc, HG_CHUNK, HG_HEADS, d).transpose(1, 0, 3, 2, 4)

    qc, kc, lc = to_chunks(qf, HG_KDIM), to_chunks(k, HG_KDIM), to_chunks(log_f, HG_KDIM)
    vc = to_chunks(i.astype(f32), HG_VDIM)
    causal = jnp.tril(jnp.ones((HG_CHUNK, HG_CHUNK), dtype=bool))[None, None, :, :, None]

    def chunk_step(state, inp):
        q_t, k_t, v_t, l_t = inp
        cum = jnp.cumsum(l_t, axis=2)
        o_inter = jnp.einsum('bhtk,bhkv->bhtv', q_t * jnp.exp(cum), state)
        rel = jnp.where(causal, cum[:, :, :, None, :] - cum[:, :, None, :, :], -jnp.inf)
        scores = jnp.einsum('bhtk,bhtsk,bhsk->bhts', q_t, jnp.exp(rel), k_t)
        o = o_inter + jnp.einsum('bhts,bhsv->bhtv', scores, v_t)
        last = cum[:, :, -1:, :]
        state = (jnp.exp(last[:, :, 0, :])[..., None] * state
                 + jnp.einsum('bhsk,bhsv->bhkv', k_t * jnp.exp(last - cum), v_t))
        return state, o

    s0 = jnp.zeros((B, HG_HEADS, HG_KDIM, HG_VDIM), f32)
    _, o = lax.scan(chunk_step, s0, (qc, kc, vc, lc))
    o = o.transpose(1, 0, 3, 2, 4).reshape(B, S, HG_HEADS, HG_VDIM)
    o = (o * lax.rsqrt(jnp.mean(o * o, axis=-1, keepdims=True) + NORM_EPS)
         * norm_g.astype(f32).reshape(HG_HEADS, HG_VDIM))
    return (o.reshape(B, S, HG_WIDTH) * jax.nn.silu(g.astype(f32))).astype(g.dtype)


def rwkv7_mixer(p, mu, w0, w2, a0, a2, g2, k_k, k_a, r_k, gn_w, gn_b):
    B, S, _ = p.shape
    dt = p.dtype
    f32 = jnp.float32
    p = p.astype(f32)
    prev = jnp.pad(p, ((0, 0), (1, 0), (0, 0)))[:, :-1]
    p = p + mu.astype(f32) * (prev - p)
    r, k, v, wd, ad, gd = jnp.split(p, RW_SPLITS, axis=-1)
    w = -jax.nn.softplus(-(w0.astype(f32) + jnp.tanh(wd) @ w2.astype(f32))) - 0.5
    decay = jnp.exp(-jnp.exp(w))
    a = jax.nn.sigmoid(a0.astype(f32) + ad @ a2.astype(f32))
    g = jax.nn.sigmoid(gd) @ g2.astype(f32)

    def heads(t):
        return t.reshape(B, S, RW_HEADS, RW_HEAD)

    kk = heads(k * k_k.astype(f32))
    kk = kk * lax.rsqrt(jnp.maximum(jnp.sum(kk * kk, axis=-1, keepdims=True), 1e-24))
    k = k * (1.0 + (a - 1.0) * k_a.astype(f32))
    r_h, k_h, v_h, w_h, a_h = heads(r), heads(k), heads(v), heads(decay), heads(a)

    def seq(t):
        return jnp.swapaxes(t, 0, 1)

    def time_step(state, inp):
        r_t, w_t, k_t, v_t, a_t, b_t = inp
        sa = jnp.einsum('bhij,bhj->bhi', state, a_t)
        state = (state * w_t[:, :, None, :] + sa[..., None] * b_t[:, :, None, :]
                 + v_t[..., None] * k_t[:, :, None, :])
        return state, jnp.einsum('bhij,bhj->bhi', state, r_t)

    s0 = jnp.zeros((B, RW_HEADS, RW_HEAD, RW_HEAD), f32)
    _, y = lax.scan(time_step, s0, (seq(r_h), seq(w_h), seq(k_h), seq(v_h), seq(-kk), seq(kk * a_h)))
    y = seq(y)
    mean = jnp.mean(y, axis=-1, keepdims=True)
    var = jnp.mean(jnp.square(y - mean), axis=-1, keepdims=True)
    y = ((y - mean) * lax.rsqrt(var + RW_GN_EPS)).reshape(B, S, RW_WIDTH) * gn_w.astype(f32) + gn_b.astype(f32)
    bonus = jnp.sum(r_h * k_h * r_k.astype(f32), axis=-1, keepdims=True) * v_h
    return ((y + bonus.reshape(B, S, RW_WIDTH)) * g).astype(dt)


def hier_moe(h, wg_r, bg_r, we_r, be_r, w_gate, w_up, w_down):
    B, S, D = h.shape
    N = B * S
    f32 = jnp.float32
    xf = h.reshape(N, D)
    lg = (xf @ wg_r).astype(f32) + bg_r.astype(f32)
    pg = jax.nn.softmax(lg, axis=-1)
    _, grp_idx = lax.top_k(lg, 1)
    p_grp = jnp.take_along_axis(pg, grp_idx, axis=1)[:, 0]
    le = ((xf @ we_r).astype(f32) + be_r.astype(f32)).reshape(N, N_GROUPS, EXPERTS_PER_GROUP)
    sel = jnp.broadcast_to(grp_idx[:, :, None], (N, 1, EXPERTS_PER_GROUP))
    pe = jax.nn.softmax(jnp.take_along_axis(le, sel, axis=1)[:, 0], axis=-1)
    top_p, top_i = lax.top_k(pe, TOP_K)
    wts = p_grp[:, None] * top_p / jnp.sum(top_p, axis=-1, keepdims=True)
    eid = (grp_idx * EXPERTS_PER_GROUP + top_i).astype(jnp.int32)

    A = N * TOP_K
    eid_flat = eid.reshape(A)
    tok_flat = jnp.repeat(jnp.arange(N, dtype=jnp.int32), TOP_K)
    w_flat = wts.reshape(A)
    sorted_e, order = lax.sort_key_val(eid_flat, jnp.arange(A, dtype=jnp.int32))
    counts = jnp.bincount(eid_flat, length=N_EXPERTS).astype(jnp.int32)
    starts = jnp.cumsum(counts) - counts
    padded = (counts + MOE_BLOCK - 1) // MOE_BLOCK * MOE_BLOCK
    pad_end = jnp.cumsum(padded)
    pad_start = pad_end - padded
    dest = pad_start[sorted_e] + (jnp.arange(A, dtype=jnp.int32) - starts[sorted_e])
    n_blocks = -(-(A + N_EXPERTS * MOE_BLOCK) // MOE_BLOCK)
    P = n_blocks * MOE_BLOCK
    buf_tok = jnp.zeros((P,), jnp.int32).at[dest].set(tok_flat[order])
    buf_w = jnp.zeros((P,), f32).at[dest].set(w_flat[order])
    blk_e = jnp.minimum(jnp.searchsorted(pad_end, jnp.arange(n_blocks, dtype=jnp.int32) * MOE_BLOCK,
                                         side='right'), N_EXPERTS - 1).astype(jnp.int32)
    xb = xf[buf_tok].reshape(n_blocks, MOE_BLOCK, D)

    def expert_block(args):
        xblk, e = args
        hid = jax.nn.silu(xblk @ w_gate[e]) * (xblk @ w_up[e])
        return hid @ w_down[e]

    yb = lax.map(expert_block, (xb, blk_e)).reshape(P, D)
    out = jnp.zeros((N, D), f32).at[buf_tok].add(yb.astype(f32) * buf_w[:, None])
    return out.reshape(B, S, D).astype(h.dtype)


def setup_inputs(seed: int = 0) -> dict:
    key = jax.random.key(seed)
    ks = iter(jax.random.split(key, 40))
    L, D = DEPTH, D_MODEL

    def nrm(shape, scale):
        return scale * jax.random.normal(next(ks), shape, jnp.float32)

    return {
        'x': nrm((BATCH, SEQ, D), 1.0),
        'c': nrm((BATCH, D), 1.0),
        'ada_w': nrm((L, D, 6 * D), 0.5 * D ** -0.5),
        'ada_b': nrm((L, 6 * D), 0.01),
        'norm1_g': 1.0 + nrm((L, D), 0.02),
        'w_in': nrm((L, D, IN_COLS), D ** -0.5),
        'hg_lb': nrm((L + 1, HG_FDIM), 0.5),
        'hg_norm_g': 1.0 + nrm((L, HG_WIDTH), 0.02),
        'rw_mu': jax.random.uniform(next(ks), (L, RW_SHIFT_COLS), jnp.float32),
        'rw_w0': jnp.linspace(-6.0, -1.0, RW_WIDTH, dtype=jnp.float32) + nrm((L, RW_WIDTH), 0.1),
        'rw_w2': nrm((L, RW_DECAY_LORA, RW_WIDTH), 0.1 * RW_DECAY_LORA ** -0.5),
        'rw_a0': nrm((L, RW_WIDTH), 0.1),
        'rw_a2': nrm((L, RW_AAA_LORA, RW_WIDTH), RW_AAA_LORA ** -0.5),
        'rw_g2': nrm((L, RW_GATE_LORA, RW_WIDTH), RW_GATE_LORA ** -0.5),
        'rw_kk': 0.85 + nrm((L, RW_WIDTH), 0.02),
        'rw_ka': 1.0 + nrm((L, RW_WIDTH), 0.02),
        'rw_rk': -0.04 + nrm((L, RW_HEADS, RW_HEAD), 0.02),
        'rw_gn_w': 1.0 + nrm((L, RW_WIDTH), 0.02),
        'rw_gn_b': nrm((L, RW_WIDTH), 0.01),
        'w_proj_a': nrm((L, HG_WIDTH, D), HG_WIDTH ** -0.5),
        'w_proj_b': nrm((L, RW_WIDTH, D), RW_WIDTH ** -0.5),
        'w_out': nrm((L, D, D), D ** -0.5),
        'norm2_g': 1.0 + nrm((L, D), 0.02),
        'router_g_w': nrm((L, D, N_GROUPS), D ** -0.5),
        'router_g_b': nrm((L, N_GROUPS), 0.01),
        'router_e_w': nrm((L, D, N_EXPERTS), D ** -0.5),
        'router_e_b': nrm((L, N_EXPERTS), 0.01),
        'exp_w_gate': nrm((L, N_EXPERTS, D, D_FF_EXPERT), D ** -0.5),
        'exp_w_up': nrm((L, N_EXPERTS, D, D_FF_EXPERT), D ** -0.5),
        'exp_w_down': nrm((L, N_EXPERTS, D_FF_EXPERT, D), D_FF_EXPERT ** -0.5),
        'final_g': 1.0 + nrm((D,), 0.02),
    }


def reference(x, c, ada_w, ada_b, norm1_g, w_in, hg_lb, hg_norm_g, rw_mu, rw_w0, rw_w2, rw_a0, rw_a2,
              rw_g2, rw_kk, rw_ka, rw_rk, rw_gn_w, rw_gn_b, w_proj_a, w_proj_b, w_out, norm2_g,
              router_g_w, router_g_b, router_e_w, router_e_b, exp_w_gate, exp_w_up, exp_w_down, final_g):
    lb_all = jnp.cumsum(jax.nn.softmax(hg_lb.astype(jnp.float32), axis=0), axis=0)
    cs = jax.nn.silu(c)
    for l in range(DEPTH):
        mod = cs @ ada_w[l] + ada_b[l]
        sh1, sc1, gt1, sh2, sc2, gt2 = jnp.split(mod[:, None, :], 6, axis=-1)
        h = rmsnorm(x, norm1_g[l]) * (1.0 + sc1) + sh1
        proj = h @ w_in[l]
        hq, hf, hi, hg, rw_p, gates = jnp.split(proj, IN_SPLITS, axis=-1)
        o_a = hgrn2_mixer(hq, hf, hi, hg, lb_all[l], hg_norm_g[l])
        o_b = rwkv7_mixer(rw_p, rw_mu[l], rw_w0[l], rw_w2[l], rw_a0[l], rw_a2[l], rw_g2[l],
                          rw_kk[l], rw_ka[l], rw_rk[l], rw_gn_w[l], rw_gn_b[l])
        gate_a, gate_b = jnp.split(jax.nn.sigmoid(gates), 2, axis=-1)
        mixed = gate_a * (o_a @ w_proj_a[l]) + gate_b * (o_b @ w_proj_b[l])
        x = x + gt1 * (mixed @ w_out[l])
        h2 = rmsnorm(x, norm2_g[l]) * (1.0 + sc2) + sh2
        x = x + gt2 * hier_moe(h2, router_g_w[l], router_g_b[l], router_e_w[l], router_e_b[l],
                               exp_w_gate[l], exp_w_up[l], exp_w_down[l])
    return rmsnorm(x, final_g)
```

```python
import numpy as np
import ml_dtypes
import concourse.bass as bass
import concourse.mybir as mybir
from concourse.bass_utils import run_bass_kernel_spmd

F32 = mybir.dt.float32
BF16 = mybir.dt.bfloat16
ALU = mybir.AluOpType
AF = mybir.ActivationFunctionType
AX = mybir.AxisListType
C0 = float(np.exp(-0.5))
D = 1024


class Prog:
    ENG = ("pe", "act", "dve", "pool", "sp")

    def __init__(self, nc):
        self.nc = nc
        self.ops = {e: [] for e in self.ENG}
        self.cnt = {}
        self.last_w = {}
        self.readers = {}
        self.seen = {e: {} for e in self.ENG}
        self.pending = {e: {} for e in self.ENG}
        self.alias = {}
        self.tot = {}
        self.last_rg = 0
        self.last_pe_tok = None
        self.cap = None
        self.nb = 6
        self.bank_i = 0

    def barrier(self):
        for e in self.ENG:
            self.pending[e] = dict(self.cnt)

    def _deps(self, eng, reads, writes):
        deps = {}

        def add(tok):
            s, v = tok
            if eng == "pe" and s.startswith("E:pe"):
                return
            if self.seen[eng].get(s, 0) >= v:
                return
            if deps.get(s, 0) < v:
                deps[s] = v

        for r in reads:
            if r in self.last_w:
                add(self.last_w[r])
        for w in writes:
            if w in self.last_w:
                add(self.last_w[w])
            for t in self.readers.get(w, ()):
                add(t)
        for s, v in self.pending[eng].items():
            if self.seen[eng].get(s, 0) < v and deps.get(s, 0) < v and not (eng == "pe" and s.startswith("E:pe")):
                deps[s] = v
        self.pending[eng] = {}
        for s, v in deps.items():
            self.seen[eng][s] = v
        return list(deps.items())

    def begin_capture(self, banks):
        self.cap = []
        self.cap_banks = list(banks)
        self.cap_bi = 0

    def end_capture(self):
        c = self.cap
        self.cap = None
        return c

    def replay_merged(self, caps):
        idx = [0] * len(caps)
        total = sum(len(c) for c in caps)
        for _ in range(total):
            best, bf = None, None
            for k, c in enumerate(caps):
                if idx[k] < len(c):
                    f = (idx[k] + 1) / len(c)
                    if bf is None or f < bf:
                        best, bf = k, f
            a = caps[best][idx[best]]
            idx[best] += 1
            self.op(*a)

    def op(self, eng, fn, reads=(), writes=(), sem=None, inc=1, rg=0):
        if self.cap is not None:
            self.cap.append((eng, fn, list(reads), list(writes), sem, inc, rg))
            return None
        reads = [self.alias.get(r, r) for r in reads]
        writes = [self.alias.get(w, w) for w in writes]
        ps_r = [r for r in reads if isinstance(r, tuple) and r[0] == "ps"]
        if ps_r:
            reads = [r for r in reads if not (isinstance(r, tuple) and r[0] == "ps")]
            writes = writes + [r for r in ps_r if r not in writes]
        deps = self._deps(eng, reads, writes)
        if eng == "pe":
            if rg != self.last_rg and self.last_pe_tok is not None:
                ls, lv = self.last_pe_tok
                if not any(d[0] == ls and d[1] >= lv for d in deps):
                    deps = [d for d in deps if d[0] != ls] + [(ls, lv)]
            self.last_rg = rg
        if sem is not None:
            s = sem
        else:
            import os
            ch = int(os.environ.get("KSEMCH", "20000"))
            tot = self.tot.get(eng, 0)
            self.tot[eng] = tot + 1
            s = "E:%s#%d" % (eng, tot // ch)
        self.cnt[s] = self.cnt.get(s, 0) + inc
        tok = (s, self.cnt[s])
        self.ops[eng].append((deps, fn, s, inc))
        if eng == "pe":
            self.last_pe_tok = tok
        for w in writes:
            self.last_w[w] = tok
            self.readers[w] = []
        for r in reads:
            self.readers.setdefault(r, []).append(tok)
        return tok

    def dma(self, eng, out, in_, key, reads=(), writes=()):
        return self.op(eng, lambda e: e.dma_start(out=out, in_=in_), reads, writes, sem="D:" + key, inc=16)

    def bank(self):
        if self.cap is not None:
            b = self.cap_banks[self.cap_bi % len(self.cap_banks)]
            self.cap_bi += 1
            return b
        b = self.bank_i
        self.bank_i = (self.bank_i + 1) % self.nb
        return b

    def emit(self, final_waits):
        nc = self.nc
        names = sorted(self.cnt.keys())
        import contextlib
        with contextlib.ExitStack() as st:
            sems = {n: st.enter_context(nc.semaphore("s%d" % i)) for i, n in enumerate(names)}
            block = st.enter_context(nc.Block())

            def run(eng):
                def f(e):
                    for deps, fn, s, inc in self.ops[eng]:
                        for ds, dv in deps:
                            e.wait_ge(sems[ds], dv)
                        fn(e).then_inc(sems[s], inc)
                    if eng == "sp":
                        for s, v in final_waits:
                            e.wait_ge(sems[s], v)
                return f

            block.tensor(run("pe"))
            block.scalar(run("act"))
            block.vector(run("dve"))
            block.gpsimd(run("pool"))
            block.sync(run("sp"))


def _consts():
    c = {}
    c["ident"] = np.eye(128, dtype=np.float32)
    bo = np.zeros((128, 128), np.float32)
    bo[:64, :64] = 1
    bo[64:, 64:] = 1
    c["bones"] = bo
    c["ones"] = np.ones((128, 128), np.float32)
    hs = np.zeros((128, 2), np.float32)
    hs[:64, 0] = 1
    hs[64:, 1] = 1
    c["hsel"] = hs
    rm = np.ones((128, 512), np.float32)
    rm[:, ::64] = 0
    c["rmask"] = rm
    s = np.arange(64)[:, None]
    t = np.arange(64)[None, :]
    le = (s <= t).astype(np.float32)
    lt = (s < t).astype(np.float32)
    gt = (s > t).astype(np.float32)
    z = np.zeros((64, 1), np.float32)

    def pad(a):
        return np.concatenate([a, np.zeros((128 - a.shape[0], a.shape[1]), np.float32)], 0)

    c["hmask"] = pad(np.tile(le, (1, 8)))
    c["nmask"] = pad(np.tile(np.concatenate([lt, le, lt, le], 1), (1, 2)))
    c["amask"] = pad(np.tile(gt, (1, 4)))
    offs = {}
    o = 0
    arrs = []
    for k, v in c.items():
        offs[k] = (o, v.shape[1])
        o += v.shape[1]
        arrs.append(v)
    return np.ascontiguousarray(np.concatenate(arrs, 1)), offs


PV_LAYOUT = [("c", 8), ("ada_b", 48), ("n1g", 8), ("n2g", 8), ("lb0", 1), ("lb1", 1),
             ("w0", 1), ("a0", 1), ("kk", 1), ("ka", 1), ("rk", 1),
             ("mu_fm", 416), ("mu_tm", 128), ("hgn", 128), ("gnw", 128), ("gnb", 128),
             ("rb", 36), ("sel", 4)]


def _pv_offs():
    o = 0
    d = {}
    for k, n in PV_LAYOUT:
        d[k] = (o, n)
        o += n
    return d, o


def build(S):
    NB = S // 512
    NT = S // 4
    NTT = NT // 128
    TG = min(NT, 1024)
    NG = NT // TG
    cst_np, co = _consts()
    pvo, NV = _pv_offs()
    NCST = cst_np.shape[1]

    nc = bass.Bass("TRN2", target_bir_lowering=False)
    P = Prog(nc)

    def din(name, shape, dt=F32):
        return nc.dram_tensor(name, shape, dt, kind="ExternalInput")

    x = din("x", [S, D])
    cst = din("cst", [128, NCST])
    pv = din("pv", [128, NV])
    ada_w = din("ada_w", [D, 6 * D])
    wfm = din("wfm", [D, 672])
    wtm = din("wtm", [D, 384])
    lora = din("lora", [128, 384])
    wgates = din("wgates", [D, 2048])
    wpa = din("wpa", [512, D])
    wpb = din("wpb", [512, D])
    wout = din("wout", [D, D])
    wr = din("wr", [D, 36])
    import os
    NE = 1 if os.environ.get("KSTOP") else 32
    eg = din("eg", [NE, D, 512])
    eu = din("eu", [NE, D, 512])
    ed = din("ed", [NE, 512, D])
    y = nc.dram_tensor("y", [NT, D], F32, kind="ExternalOutput")
    xin = [nc.dram_tensor("xin%d" % q, [NT, D], BF16) for q in range(4)]
    xout = [nc.dram_tensor("xout%d" % q, [NT, D], BF16) for q in range(4)]
    x1s = nc.dram_tensor("x1s", [NT, D], F32)

    tcount = [0]
    import contextlib
    stP = contextlib.ExitStack()
    cur = [stP]

    def sb(shape, dt=F32, name=None):
        tcount[0] += 1
        return cur[0].enter_context(nc.sbuf_tensor(name or ("t%d" % tcount[0]), list(shape), dt))

    psum = nc.alloc_psum_tensor("psum", [128, 8, 512], F32)

    def PB(b):
        return psum[:, b, :]

    def PBbf(b):
        return psum[:, b, :].bitcast(BF16)

    def bk(b):
        return ("ps", b)

    def rsq(out, in_, rk, wk):
        P.op("act", lambda e: e.activation(out=out, in_=in_, func=AF.Sqrt), rk, wk)
        P.op("dve", lambda e: e.reciprocal(out=out, in_=out), wk, wk)

    CST = sb([128, NCST])
    PV = sb([128, NV])
    P.dma("sp", CST[:, :], cst[:, :], "cst", writes=["CST"])
    P.dma("sp", PV[:, :], pv[:, :], "pv", writes=["PV"])

    def cs(k, rows=128):
        o, n = co[k]
        return CST[0:rows, o:o + n]

    def pvs(k, rows=128):
        o, n = pvo[k]
        return PV[0:rows, o:o + n]

    identb = sb([128, 128], BF16)
    P.op("dve", lambda e: e.tensor_copy(out=identb[:, :], in_=cs("ident")), ["CST"], ["identb"])

    csil = sb([128, 8])
    P.op("act", lambda e: e.activation(out=csil[:, :], in_=pvs("c"), func=AF.Silu), ["PV"], ["csil"])
    mod = sb([128, 48])
    s1 = sb([128, 8])
    s2 = sb([128, 8])
    lbv = sb([128, 4])
    stA = contextlib.ExitStack()
    cur[0] = stA
    W1f = sb([128, 8, 672], BF16)
    W2f = sb([128, 8, 416], BF16)
    W1t = sb([128, 8, 384], BF16)
    W2t = sb([128, 8, 128], BF16)
    lorb = sb([128, 384], BF16)
    stS = contextlib.ExitStack()
    cur[0] = stS
    stg = [sb([128, 8, 512]) for _ in range(2)]
    tmpw = sb([128, 512])
    lor32 = sb([128, 384])
    cur[0] = stA
    adst = stg
    mb = P.bank()
    for g in range(12):
        t = adst[g % 2]
        k = "stg%d" % (g % 2)
        P.dma("sp" if g % 2 == 0 else "act", t[:, :, :],
              ada_w[:, g * 512:(g + 1) * 512].rearrange("(kc p) n -> p kc n", p=128), k, writes=[k])
        for cc in range(4):
            j = g * 4 + cc
            for kc in range(8):
                P.op("pe", lambda e, t=t, cc=cc, kc=kc, j=j: e.matmul(
                    PB(mb)[:, j:j + 1], lhsT=t[:, kc, cc * 128:(cc + 1) * 128], rhs=csil[:, kc:kc + 1],
                    start=(kc == 0), stop=(kc == 7)), [k, "csil"], [bk(mb)])
    P.op("dve", lambda e: e.tensor_tensor(out=mod[:, :], in0=PB(mb)[:, 0:48], in1=pvs("ada_b"), op=ALU.add),
         [bk(mb), "PV"], ["mod"])
    P.op("dve", lambda e: e.scalar_tensor_tensor(out=s1[:, :], in0=mod[:, 8:16], scalar=1.0, in1=pvs("n1g"),
                                                 op0=ALU.add, op1=ALU.mult), ["mod", "PV"], ["s1"])
    P.op("dve", lambda e: e.scalar_tensor_tensor(out=s2[:, :], in0=mod[:, 32:40], scalar=1.0, in1=pvs("n2g"),
                                                 op0=ALU.add, op1=ALU.mult), ["mod", "PV"], ["s2"])
    P.op("dve", lambda e: e.tensor_tensor(out=lbv[:, 0:1], in0=pvs("lb0"), in1=pvs("lb1"), op=ALU.subtract),
         ["PV"], ["lbv"])
    P.op("act", lambda e: e.activation(out=lbv[:, 1:2], in_=lbv[:, 0:1], func=AF.Sigmoid), ["lbv"], ["lbv"])
    P.op("dve", lambda e: e.tensor_scalar(out=lbv[:, 2:3], in0=lbv[:, 1:2], scalar1=-1.0, scalar2=1.0,
                                          op0=ALU.mult, op1=ALU.add), ["lbv"], ["lbv"])
    P.op("dve", lambda e: e.tensor_scalar(out=lbv[:, 3:4], in0=pvs("ka"), scalar1=-1.0, scalar2=1.0,
                                          op0=ALU.mult, op1=ALU.add), ["PV", "lbv"], ["lbv"])
    LB, OML, OMK = lbv[:, 1:2], lbv[:, 2:3], lbv[:, 3:4]

    def stage(i):
        return stg[i % 2], "stg%d" % (i % 2)

    si = [0]

    def load_cast(dst_fn, src_ap, ncols, mu=None, dst2_fn=None, eng_dma="sp"):
        t, k = stage(si[0])
        si[0] += 1
        P.dma(eng_dma, t[:, :, 0:ncols], src_ap.rearrange("(kc p) n -> p kc n", p=128), k, writes=[k])
        for kc in range(8):
            if mu is None:
                P.op("act" if kc % 2 else "dve",
                     (lambda e, kc=kc: e.activation(out=dst_fn(kc), in_=t[:, kc, 0:ncols], func=AF.Copy)) if kc % 2 else
                     (lambda e, kc=kc: e.tensor_copy(out=dst_fn(kc), in_=t[:, kc, 0:ncols])),
                     [k], ["Wa"])
            else:
                P.op("dve", lambda e, kc=kc: e.tensor_tensor(out=tmpw[:, 0:ncols], in0=t[:, kc, 0:ncols], in1=mu,
                                                             op=ALU.mult), [k, "PV"], ["tmpw"])
                P.op("act", lambda e, kc=kc: e.activation(out=dst2_fn(kc), in_=tmpw[:, 0:ncols], func=AF.Copy),
                     ["tmpw"], ["Wa"])
                P.op("dve", lambda e, kc=kc: e.tensor_tensor(out=dst_fn(kc), in0=t[:, kc, 0:ncols],
                                                             in1=tmpw[:, 0:ncols], op=ALU.subtract),
                     [k, "tmpw"], ["Wa"])

    load_cast(lambda kc: W1f[:, kc, 0:256], wfm[:, 0:256], 256)
    load_cast(lambda kc: W1f[:, kc, 256:672], wfm[:, 256:672], 416, mu=pvs("mu_fm"),
              dst2_fn=lambda kc: W2f[:, kc, :], eng_dma="act")
    load_cast(lambda kc: W1t[:, kc, 0:256], wtm[:, 0:256], 256)
    load_cast(lambda kc: W1t[:, kc, 256:384], wtm[:, 256:384], 128, mu=pvs("mu_tm"),
              dst2_fn=lambda kc: W2t[:, kc, :], eng_dma="act")
    P.dma("sp", lor32[:, :], lora[:, :], "lora", writes=["lor32"])
    P.op("dve", lambda e: e.tensor_copy(out=lorb[:, :], in_=lor32[:, :]), ["lor32"], ["Wa"])
    P.barrier()
    stS.close()

    xsl = [sb([128, D]) for _ in range(2)]
    NF = {"xn": [sb([128, D], BF16) for _ in range(2)], "stat": [sb([128, 4]) for _ in range(2)]}
    nti = [0]

    def load_x_tile(row0):
        i = nti[0] % 2
        P.dma("sp", xsl[i][:, :], x[row0:row0 + 128, :], "x%d" % i, writes=["xs%d" % i])
        return xsl[i], "xs%d" % i

    def norm_fm(xt, xk, sc, bi, sck, out_fn, outk):
        i = nti[0] % 2
        nti[0] += 1
        xn, stat = NF["xn"], NF["stat"]
        junk = xn[i]
        st_, sk = stat[i], "stat%d" % i
        P.op("act", lambda e: e.activation(out=junk[:, :], in_=xt[:, :], func=AF.Square, accum_out=st_[:, 0:1]),
             [xk], ["xn%d" % i, sk])
        P.op("dve", lambda e: e.tensor_scalar(out=st_[:, 1:2], in0=st_[:, 0:1], scalar1=1.0 / D, scalar2=1e-6,
                                              op0=ALU.mult, op1=ALU.add), [sk], [sk])
        rsq(st_[:, 2:3], st_[:, 1:2], [sk], [sk])
        xnt, xnk = xn[i], "xn%d" % i
        P.op("dve", lambda e: e.tensor_scalar(out=xnt[:, :], in0=xt[:, :], scalar1=st_[:, 2:3], scalar2=None,
                                              op0=ALU.mult), [xk, sk], [xnk])
        b = P.bank()
        for kc in range(8):
            P.op("pe", lambda e, kc=kc: e.transpose(out=PBbf(b)[:, kc * 128:(kc + 1) * 128],
                                                    in_=xnt[:, kc * 128:(kc + 1) * 128], identity=identb[:, :]),
                 [xnk, "identb"], [bk(b)])
        for kc in range(8):
            if kc % 2:
                P.op("act", lambda e, kc=kc: e.activation(out=out_fn(kc), in_=PBbf(b)[:, kc * 128:(kc + 1) * 128],
                                                          func=AF.Identity, scale=sc[:, kc:kc + 1],
                                                          bias=bi[:, kc:kc + 1]), [bk(b), sck], [outk])
            else:
                P.op("dve", lambda e, kc=kc: e.tensor_scalar(out=out_fn(kc), in0=PBbf(b)[:, kc * 128:(kc + 1) * 128],
                                                             scalar1=sc[:, kc:kc + 1], scalar2=bi[:, kc:kc + 1],
                                                             op0=ALU.mult, op1=ALU.add), [bk(b), sck], [outk])
        return st_, sk

    hTs = [sb([128, 8, 512], BF16)] * 2
    hSh = [sb([128, 8, 512], BF16)] * 2
    BIG = lambda dt=F32: sb([128, 512], dt)
    h_f, h_lf, h_kk, h_q, h_cum, h_e, h_t = BIG(), BIG(), BIG(), BIG(), BIG(), BIG(), BIG()
    hq2, hf2 = BIG(), BIG()
    h_qhat, h_qtl, h_ktl, h_khat = BIG(BF16), BIG(BF16), BIG(BF16), BIG(BF16)
    h_khT = sb([64, 8, 128], BF16)
    h_scT = sb([64, 8, 64], BF16)
    VhL = [sb([64, 8, 128], BF16) for _ in range(2)]
    SGhL = [sb([64, 8, 128], BF16) for _ in range(2)]
    VrL = [sb([64, 8, 128], BF16) for _ in range(2)]
    S32 = sb([128, 128])
    Sbf = [sb([128, 128], BF16) for _ in range(2)]
    h_o = sb([64, 8, 128])
    h_sq = sb([64, 8, 128])
    h_st = sb([64, 32])
    r_tw, r_ad, r_sg = sb([32, 512], BF16), sb([32, 512], BF16), sb([96, 512], BF16)
    r_sw, r_a, r_cw, r_cx, r_dl, r_Ea, r_El = h_f, h_lf, h_kk, h_q, h_cum, h_e, h_t
    for a_, b_ in (("r_sw", "h_f"), ("r_a", "h_lf"), ("r_cw", "h_kk"), ("r_cx", "h_q"), ("r_dl", "h_cum"),
                   ("r_Ea", "h_e"), ("r_El", "h_t"), ("r_t1", "r_sq"), ("r_bv", "r_kkv")):
        P.alias[a_] = b_
    r_Er, r_En = BIG(), BIG()
    r_k, r_r, r_kkv, r_sq, r_kkn, r_k2, r_rt32 = (BIG() for _ in range(7))
    r_prod = r_k
    P.alias["r_prod"] = "r_k"
    r_t1, r_bv = r_sq, r_kkv
    r_bh, r_kh = BIG(BF16), BIG(BF16)
    FM = sb([128, 8, 4, 64], BF16)
    TMa = sb([64, 8, 128], BF16)
    TMbL = [sb([64, 8, 2, 128], BF16) for _ in range(2)]
    bonL = [sb([64, 8, 2]) for _ in range(2)]
    GtL = [sb([64, 8, 128], BF16) for _ in range(2)]
    NmL = [sb([64, 4, 4, 64], BF16) for _ in range(4)]
    AkpL = [[sb([64, 4, 64], BF16) for _ in range(2)] for _ in range(4)]
    NkpL = [[sb([64, 4, 64], BF16) for _ in range(2)] for _ in range(4)]
    Z32L = [sb([64, 4, 128]) for _ in range(4)]
    ZbfL = [sb([64, 4, 128], BF16) for _ in range(4)]
    RhTL = [sb([128, 2, 64], BF16) for _ in range(4)]
    MTL = [sb([128, 2, 64], BF16) for _ in range(4)]
    H32 = sb([128, 64])
    Hbf = [sb([128, 64], BF16) for _ in range(2)]
    Yt = sb([64, 8, 128])
    r_ysq = h_sq
    P.alias["r_ysq"] = "h_sq"
    r_st = sb([64, 80])
    r_tmp = h_o
    P.alias["r_tmp"] = "h_o"
    osel = sb([64, 8, 4, 256], BF16)

    P.op("pool", lambda e: e.memset(S32[:, :], 0.0), [], ["S32"])
    P.op("pool", lambda e: e.memset(Sbf[0][:, :], 0.0), [], ["Sbf0"])
    P.op("pool", lambda e: e.memset(H32[:, :], 0.0), [], ["H32"])
    P.op("pool", lambda e: e.memset(Hbf[0][:, :], 0.0), [], ["Hbf0"])
    def v3(t, n=64):
        return t[:, :].rearrange("p (c t) -> p c t", t=n)

    state = {"sidx": 0, "hidx": 0}

    import os
    KSTOP = os.environ.get("KSTOP", "")

    class _Stop(Exception):
        pass

    def chk(tag):
        if KSTOP == tag:
            raise _Stop()

    RDGL = [sb([128, 8]) for _ in range(2)]
    HDGL = [sb([128, 8]) for _ in range(2)]
    EXTRA = {"cap": None}

    def phaseA_out(blk):
        ob_banks = [6, 7]
        par = blk % 2
        SGh, Vr, Gt, bon = SGhL[par], VrL[par], GtL[par], bonL[par]
        kSGh, kVr, kGt, kbon = "SGh%d" % par, "Vr%d" % par, "Gt%d" % par, "bon%d" % par
        Vh, TMb = VhL[par], TMbL[par]
        kVh, kTMb, kHdg, kRdg = "Vh%d" % par, "TMb%d" % par, "h_dg%d" % par, "r_dg%d" % par
        chk("A5")
        for half in range(2):
            P.op("act", lambda e, half=half: e.activation(
                out=h_o[:, half * 4:half * 4 + 4, :].rearrange("p c k -> p (c k)"), in_=PB(ob_banks[half])[0:64, :],
                func=AF.Copy), [bk(ob_banks[half])], ["h_o"])
        P.op("pool", lambda e: e.tensor_tensor(out=h_sq[:, :, :], in0=h_o[:, :, :], in1=h_o[:, :, :], op=ALU.mult),
             ["h_o"], ["h_sq"])
        P.op("dve", lambda e: e.tensor_reduce(out=h_st[:, 0:8], in_=h_sq[:, :, :], axis=AX.X, op=ALU.add), ["h_sq"], ["h_st"])
        P.op("dve", lambda e: e.tensor_scalar(out=h_st[:, 8:16], in0=h_st[:, 0:8], scalar1=1.0 / 128, scalar2=1e-6,
                                              op0=ALU.mult, op1=ALU.add), ["h_st"], ["h_st"])
        rsq(h_st[:, 16:24], h_st[:, 8:16], ["h_st"], ["h_st"])
        P.op("dve", lambda e: e.tensor_tensor(out=h_o[:, :, :], in0=h_o[:, :, :],
                                              in1=h_st[:, 16:24].unsqueeze(2).to_broadcast([64, 8, 128]), op=ALU.mult),
             ["h_o", "h_st"], ["h_o"])
        P.op("pool", lambda e: e.tensor_tensor(out=h_sq[:, :, :], in0=SGh[:, :, :],
                                               in1=pvs("hgn", 64).unsqueeze(1).to_broadcast([64, 8, 128]), op=ALU.mult),
             [kSGh, "PV", "h_st"], ["h_sq"])
        P.op("dve", lambda e: e.tensor_tensor(out=h_o[:, :, :], in0=h_o[:, :, :], in1=h_sq[:, :, :], op=ALU.mult),
             ["h_o", "h_sq"], ["h_o"])
        q = (blk * 512) // NT
        r0 = blk * 512 - q * NT
        selb = pvs("sel", 64).unsqueeze(1).unsqueeze(3).to_broadcast([64, 8, 4, 128])
        P.op("dve", lambda e: e.tensor_tensor(out=osel[:, :, :, 0:128], in0=h_o[:, :, :].unsqueeze(2).to_broadcast([64, 8, 4, 128]),
                                              in1=selb, op=ALU.mult), ["h_o", "PV"], ["osel"])

        chk("A6")
        Y4 = Yt[:, :, :].rearrange("p c (h k) -> p (c h) k", k=64)
        P.op("dve", lambda e: e.tensor_reduce(out=r_st[:, 0:16], in_=Y4, axis=AX.X, op=ALU.add), ["Yt"], ["r_st"])
        P.op("pool", lambda e: e.tensor_tensor(out=r_ysq[:, :, :], in0=Yt[:, :, :], in1=Yt[:, :, :], op=ALU.mult),
             ["Yt"], ["r_ysq"])
        P.op("dve", lambda e: e.tensor_reduce(out=r_st[:, 16:32], in_=r_ysq[:, :, :].rearrange("p c (h k) -> p (c h) k", k=64),
                                              axis=AX.X, op=ALU.add), ["r_ysq"], ["r_st"])
        P.op("dve", lambda e: e.tensor_scalar(out=r_st[:, 0:16], in0=r_st[:, 0:16], scalar1=1.0 / 64, scalar2=None,
                                              op0=ALU.mult), ["r_st"], ["r_st"])
        P.op("dve", lambda e: e.tensor_tensor(out=r_st[:, 32:48], in0=r_st[:, 0:16], in1=r_st[:, 0:16], op=ALU.mult),
             ["r_st"], ["r_st"])
        P.op("dve", lambda e: e.scalar_tensor_tensor(out=r_st[:, 48:64], in0=r_st[:, 16:32], scalar=1.0 / 64,
                                                     in1=r_st[:, 32:48], op0=ALU.mult, op1=ALU.subtract),
             ["r_st"], ["r_st"])
        P.op("dve", lambda e: e.tensor_scalar(out=r_st[:, 64:80], in0=r_st[:, 48:64], scalar1=64e-5, scalar2=None,
                                              op0=ALU.add), ["r_st"], ["r_st"])
        rsq(r_st[:, 64:80], r_st[:, 64:80], ["r_st"], ["r_st"])
        T4 = r_tmp[:, :, :].rearrange("p c (h k) -> p (c h) k", k=64)
        P.op("dve", lambda e: e.tensor_tensor(out=T4, in0=Y4, in1=r_st[:, 0:16].unsqueeze(2).to_broadcast([64, 16, 64]),
                                              op=ALU.subtract), ["Yt", "r_st"], ["r_tmp"])
        P.op("dve", lambda e: e.tensor_tensor(out=T4, in0=T4, in1=r_st[:, 64:80].unsqueeze(2).to_broadcast([64, 16, 64]),
                                              op=ALU.mult), ["r_tmp", "r_st"], ["r_tmp"])
        P.op("pool", lambda e: e.tensor_tensor(out=r_tmp[:, :, :], in0=r_tmp[:, :, :],
                                               in1=pvs("gnw", 64).unsqueeze(1).to_broadcast([64, 8, 128]), op=ALU.mult),
             ["r_tmp", "PV"], ["r_tmp"])
        P.op("pool", lambda e: e.tensor_tensor(out=r_tmp[:, :, :], in0=r_tmp[:, :, :],
                                               in1=pvs("gnb", 64).unsqueeze(1).to_broadcast([64, 8, 128]), op=ALU.add),
             ["r_tmp", "PV"], ["r_tmp"])
        Q4 = r_ysq[:, :, :].rearrange("p c (h k) -> p (c h) k", k=64)
        P.op("dve", lambda e: e.tensor_tensor(out=Q4, in0=Vr[:, :, :].rearrange("p c (h k) -> p (c h) k", k=64),
                                              in1=bon[:, :, :].rearrange("p c h -> p (c h)").unsqueeze(2).to_broadcast([64, 16, 64]),
                                              op=ALU.mult), [kVr, kbon, "r_st"], ["r_ysq"])
        P.op("dve", lambda e: e.tensor_tensor(out=r_tmp[:, :, :], in0=r_tmp[:, :, :], in1=r_ysq[:, :, :], op=ALU.add),
             ["r_tmp", "r_ysq"], ["r_tmp"])
        P.op("dve", lambda e: e.tensor_tensor(out=r_tmp[:, :, :], in0=r_tmp[:, :, :], in1=Gt[:, :, :], op=ALU.mult),
             ["r_tmp", kGt], ["r_tmp"])
        P.op("dve", lambda e: e.tensor_tensor(out=osel[:, :, :, 128:256], in0=r_tmp[:, :, :].unsqueeze(2).to_broadcast([64, 8, 4, 128]),
                                              in1=selb, op=ALU.mult), ["r_tmp", "PV"], ["osel"])
        PS = min(512, NT)
        for p_ in range(512 // PS):
            tok0 = blk * 512 + p_ * PS
            q = tok0 // NT
            r0 = tok0 - q * NT
            cA, cB = p_ * PS // 64, (p_ + 1) * PS // 64
            P.dma("sp", xin[q][r0:r0 + PS, :].rearrange("(c t) v -> t c v", t=64),
                  osel[:, cA:cB, :, :].rearrange("p c s v -> p c (s v)"), "ost", reads=["osel"], writes=["xin%d" % q])
        return q

    def phaseA_p4(blk):
        ob_banks = [6, 7]
        par = blk % 2
        Vr, Vh, TMb = VrL[par], VhL[par], TMbL[par]
        kVr, kVh, kTMb, kHdg, kRdg = "Vr%d" % par, "Vh%d" % par, "TMb%d" % par, "h_dg%d" % par, "r_dg%d" % par
        h_dg, r_dg = HDGL[par], RDGL[par]
        for pg in range(4):
            c0 = pg * 2
            Nm, nmk = NmL[pg], "Nm%d" % pg
            Zbf, zbk = ZbfL[pg], "Zbf%d" % pg
            RhT, rhk, MT, mtk = RhTL[pg], "RhT%d" % pg, MTL[pg], "MT%d" % pg
            by = 3
            for cc in range(2):
                c = c0 + cc
                si_ = state["sidx"]
                Sb, Sk = Sbf[si_ % 2], "Sbf%d" % (si_ % 2)
                Sb2, Sk2 = Sbf[(si_ + 1) % 2], "Sbf%d" % ((si_ + 1) % 2)
                state["sidx"] += 1
                obk = ob_banks[c // 4]
                oreg = PB(obk)[0:64, (c % 4) * 128:(c % 4 + 1) * 128]
                P.op("pe", lambda e, c=c, oreg=oreg: e.matmul(oreg, lhsT=h_scT[:, c, :], rhs=Vh[:, c, :], start=True, stop=False),
                     ["h_scT", kVh], [bk(obk)])
                P.op("pe", lambda e, c=c, oreg=oreg, Sb=Sb: e.matmul(oreg, lhsT=h_qhat[:, c * 64:(c + 1) * 64], rhs=Sb[:, :],
                                                                     start=False, stop=True), ["h_qhat", Sk], [bk(obk)])
                bS = 4
                P.op("pe", lambda e, c=c, bS=bS: e.matmul(PB(bS)[:, 0:128], lhsT=h_khT[:, c, :], rhs=Vh[:, c, :],
                                                          start=True, stop=True), ["h_khT", kVh], [bk(bS)])
                P.op("dve", lambda e, c=c, bS=bS: e.scalar_tensor_tensor(out=S32[:, :], in0=S32[:, :], scalar=h_dg[:, c:c + 1],
                                                                         in1=PB(bS)[:, 0:128], op0=ALU.mult, op1=ALU.add),
                     ["S32", kHdg, bk(bS)], ["S32"])
                P.op("act", lambda e, Sb2=Sb2: e.activation(out=Sb2[:, :], in_=S32[:, :], func=AF.Copy), ["S32"], [Sk2])
                hi_ = state["hidx"]
                Hb, Hk = Hbf[hi_ % 2], "Hbf%d" % (hi_ % 2)
                Hb2, Hk2 = Hbf[(hi_ + 1) % 2], "Hbf%d" % ((hi_ + 1) % 2)
                state["hidx"] += 1
                bH = 5
                for h in range(2):
                    u = cc * 2 + h
                    ph = slice(h * 64, (h + 1) * 64)
                    yreg = PB(by)[0:64, cc * 128 + h * 64:cc * 128 + (h + 1) * 64]
                    P.op("pe", lambda e, u=u, yreg=yreg, Nm=Nm, Zbf=Zbf: e.matmul(yreg, lhsT=Nm[:, u, 1, :], rhs=Zbf[:, u, 64:128],
                                                                                  start=True, stop=False), [nmk, zbk], [bk(by)])
                    P.op("pe", lambda e, u=u, yreg=yreg, c=c, h=h, Nm=Nm: e.matmul(yreg, lhsT=Nm[:, u, 3, :],
                                                                                   rhs=Vr[:, c, h * 64:(h + 1) * 64],
                                                                                   start=False, stop=False), [nmk, kVr], [bk(by)])
                    P.op("pe", lambda e, yreg=yreg, ph=ph, cc=cc, Hb=Hb, RhT=RhT: e.matmul(yreg, lhsT=RhT[ph, cc, :], rhs=Hb[ph, :],
                                                                                           start=False, stop=True),
                         [rhk, Hk], [bk(by)], rg=h * 64)
                    hreg = PB(bH)[ph, 0:64]
                    P.op("pe", lambda e, u=u, hreg=hreg, c=c, h=h, Zbf=Zbf: e.matmul(hreg, lhsT=TMb[:, c, 0, h * 64:(h + 1) * 64],
                                                                                     rhs=Zbf[:, u, 64:128], start=True, stop=False),
                         [kTMb, zbk], [bk(bH)])
                    P.op("pe", lambda e, hreg=hreg, c=c, h=h: e.matmul(hreg, lhsT=TMb[:, c, 1, h * 64:(h + 1) * 64],
                                                                       rhs=Vr[:, c, h * 64:(h + 1) * 64], start=False, stop=False),
                         [kTMb, kVr], [bk(bH)])
                    P.op("pe", lambda e, hreg=hreg, ph=ph, cc=cc, Hb=Hb, MT=MT: e.matmul(hreg, lhsT=MT[ph, cc, :], rhs=Hb[ph, :],
                                                                                         start=False, stop=True),
                         [mtk, Hk], [bk(bH)], rg=h * 64)
                P.op("dve", lambda e, c=c, bH=bH: e.scalar_tensor_tensor(out=H32[:, :], in0=H32[:, :], scalar=r_dg[:, c:c + 1],
                                                                         in1=PB(bH)[:, 0:64], op0=ALU.mult, op1=ALU.add),
                     ["H32", kRdg, bk(bH)], ["H32"])
                P.op("act", lambda e, Hb2=Hb2: e.activation(out=Hb2[:, :], in_=H32[:, :], func=AF.Copy), ["H32"], [Hk2])
            P.op("act", lambda e, by=by, c0=c0: e.activation(out=Yt[:, c0:c0 + 2, :].rearrange("p c k -> p (c k)"),
                                                             in_=PB(by)[0:64, 0:256], func=AF.Copy), [bk(by)], ["Yt"])

    def phaseA_block(blk, part):
        ob_banks = [6, 7]
        par = blk % 2
        SGh, Vr, Gt, bon = SGhL[par], VrL[par], GtL[par], bonL[par]
        kSGh, kVr, kGt, kbon = "SGh%d" % par, "Vr%d" % par, "Gt%d" % par, "bon%d" % par
        Vh, TMb = VhL[par], TMbL[par]
        kVh, kTMb, kHdg, kRdg = "Vh%d" % par, "TMb%d" % par, "h_dg%d" % par, "r_dg%d" % par
        hi = 0
        hT, hk = hTs[hi], "hT%d" % hi
        hS, shk = hSh[hi], "hS%d" % hi
        if part == "norm":
            if blk == 0:
                P.op("pool", lambda e: e.memset(hS[:, :, 0:1], 0.0), [], [shk])
            else:
                P.op("pool", lambda e: e.tensor_copy(out=hS[:, :, 0:1], in_=hT[:, :, 511:512]), [hk], [shk])
            for tt in range(4):
                xt, xk = load_x_tile(blk * 512 + tt * 128)
                norm_fm(xt, xk, s1, mod, "s1", lambda kc, tt=tt: hT[:, kc, tt * 128:(tt + 1) * 128], hk)
            P.op("act", lambda e: e.activation(out=hS[:, 0:4, 1:512], in_=hT[:, 0:4, 0:511], func=AF.Copy), [hk, shk], [shk])
            P.op("dve", lambda e: e.tensor_copy(out=hS[:, 4:8, 1:512], in_=hT[:, 4:8, 0:511]), [hk, shk], [shk])
            return

        def proj_fm(off, n, shifted):
            b = P.bank()
            nmm = 16 if shifted else 8
            i = 0
            for kc in range(8):
                P.op("pe", lambda e, kc=kc, i=i: e.matmul(PB(b)[0:n, :], lhsT=W1f[:, kc, off:off + n],
                                                          rhs=hT[:, kc, :], start=(i == 0), stop=(i == nmm - 1)),
                     [hk, "Wa"], [bk(b)])
                i += 1
                if shifted:
                    P.op("pe", lambda e, kc=kc, i=i: e.matmul(PB(b)[0:n, :], lhsT=W2f[:, kc, off - 256:off - 256 + n],
                                                              rhs=hS[:, kc, :], start=False, stop=(i == nmm - 1)),
                         [shk, "Wa"], [bk(b)])
                    i += 1
            return b

        if part == "front2":
            chk("A1")
            KSUB = int(os.environ.get("KSUB", "9"))
            for c in range(8):
                b = P.bank()
                for kc in range(8):
                    P.op("pe", lambda e, kc=kc, c=c, b=b: e.matmul(PB(b)[0:64, 0:384], lhsT=hT[:, kc, c * 64:64 + c * 64],
                                                                   rhs=W1t[:, kc, :], start=(kc == 0), stop=False),
                         [hk, "Wa"], [bk(b)])
                if KSUB >= 2:
                    for kc in range(8):
                        P.op("pe", lambda e, kc=kc, c=c, b=b: e.matmul(PB(b)[0:64, 256:384], lhsT=hS[:, kc, c * 64:64 + c * 64],
                                                                       rhs=W2t[:, kc, :], start=False, stop=(kc == 7)),
                             [shk, "Wa"], [bk(b)])
                if KSUB >= 3:
                    P.op("dve", lambda e, c=c, b=b: e.tensor_copy(out=Vh[:, c, :], in_=PB(b)[0:64, 0:128]), [bk(b)], [kVh])
                if KSUB >= 4:
                    P.op("act", lambda e, c=c, b=b: e.activation(out=SGh[:, c, :], in_=PB(b)[0:64, 128:256], func=AF.Silu),
                         [bk(b)], [kSGh])
                if KSUB >= 5:
                    P.op("dve", lambda e, c=c, b=b: e.tensor_copy(out=Vr[:, c, :], in_=PB(b)[0:64, 256:384]), [bk(b)], [kVr])

            chk("A3")
            br = proj_fm(256, 128, True)
            P.op("act", lambda e: e.activation(out=r_r[:, :], in_=PB(br)[:, :], func=AF.Copy), [bk(br)], ["r_r"])
            bkk = proj_fm(384, 128, True)
            P.op("act", lambda e: e.activation(out=r_k[:, :], in_=PB(bkk)[:, :], func=AF.Copy), [bk(bkk)], ["r_k"])
            P.op("dve", lambda e: e.tensor_scalar(out=r_kkv[:, :], in0=PB(bkk)[:, :], scalar1=pvs("kk"), scalar2=None,
                                                  op0=ALU.mult), [bk(bkk), "PV"], ["r_kkv"])
            bwd = proj_fm(512, 32, True)
            P.op("act", lambda e: e.activation(out=r_tw[:, :], in_=PB(bwd)[0:32, :], func=AF.Tanh), [bk(bwd)], ["r_tw"])
            bad = proj_fm(544, 32, True)
            P.op("dve", lambda e: e.tensor_copy(out=r_ad[:, :], in_=PB(bad)[0:32, :]), [bk(bad)], ["r_ad"])
            bgd = proj_fm(576, 96, True)
            P.op("act", lambda e: e.activation(out=r_sg[:, :], in_=PB(bgd)[0:96, :], func=AF.Sigmoid), [bk(bgd)], ["r_sg"])
            bW = P.bank()
            P.op("pe", lambda e: e.matmul(PB(bW)[:, :], lhsT=lorb[0:32, 0:128], rhs=r_tw[:, :], start=True, stop=True),
                 ["Wa", "r_tw"], [bk(bW)])
            P.op("act", lambda e: e.activation(out=r_sw[:, :], in_=PB(bW)[:, :], func=AF.Sigmoid, bias=pvs("w0")),
                 [bk(bW), "PV"], ["r_sw"])
            bA = P.bank()
            P.op("pe", lambda e: e.matmul(PB(bA)[:, :], lhsT=lorb[0:32, 128:256], rhs=r_ad[:, :], start=True, stop=True),
                 ["Wa", "r_ad"], [bk(bA)])
            P.op("act", lambda e: e.activation(out=r_a[:, :], in_=PB(bA)[:, :], func=AF.Sigmoid, bias=pvs("a0")),
                 [bk(bA), "PV"], ["r_a"])
            P.op("pool", lambda e: e.tensor_tensor(out=r_sq[:, :], in0=r_kkv[:, :], in1=r_kkv[:, :], op=ALU.mult),
                 ["r_kkv"], ["r_sq"])
            bN = P.bank()
            P.op("pe", lambda e: e.matmul(PB(bN)[:, :], lhsT=cs("bones"), rhs=r_sq[:, :], start=True, stop=True),
                 ["CST", "r_sq"], [bk(bN)])
            P.op("dve", lambda e: e.tensor_scalar(out=r_sq[:, :], in0=PB(bN)[:, :], scalar1=1e-24, scalar2=None,
                                                  op0=ALU.max), [bk(bN)], ["r_sq"])
            rsq(r_sq[:, :], r_sq[:, :], ["r_sq"], ["r_sq"])
            P.op("dve", lambda e: e.tensor_tensor(out=r_kkn[:, :], in0=r_kkv[:, :], in1=r_sq[:, :], op=ALU.mult),
                 ["r_kkv", "r_sq"], ["r_kkn"])
            P.op("dve", lambda e: e.tensor_scalar(out=r_t1[:, :], in0=r_a[:, :], scalar1=pvs("ka"), scalar2=OMK,
                                                  op0=ALU.mult, op1=ALU.add), ["r_a", "PV", "lbv"], ["r_t1"])
            P.op("pool", lambda e: e.tensor_tensor(out=r_k2[:, :], in0=r_k[:, :], in1=r_t1[:, :], op=ALU.mult),
                 ["r_k", "r_t1"], ["r_k2"])
            P.op("pool", lambda e: e.tensor_tensor(out=r_bv[:, :], in0=r_kkn[:, :], in1=r_a[:, :], op=ALU.mult),
                 ["r_kkn", "r_a"], ["r_bv"])
            P.op("dve", lambda e: e.tensor_tensor_scan(out=r_cw[:, :], data0=cs("rmask"), data1=r_sw[:, :], initial=0.0,
                                                       op0=ALU.mult, op1=ALU.add), ["r_sw", "CST"], ["r_cw"])
            P.op("pool", lambda e: e.tensor_tensor(out=r_cx[:, :], in0=r_cw[:, :], in1=r_sw[:, :], op=ALU.subtract),
                 ["r_cw", "r_sw"], ["r_cx"])
            P.op("dve", lambda e: e.tensor_tensor(out=v3(r_dl), in0=v3(r_cw)[:, :, 63:64].to_broadcast([128, 8, 64]),
                                                  in1=v3(r_cw), op=ALU.subtract), ["r_cw"], ["r_dl"])
            P.op("act", lambda e: e.activation(out=r_Er[:, :], in_=r_cw[:, :], func=AF.Exp, scale=-C0), ["r_cw"], ["r_Er"])
            P.op("act", lambda e: e.activation(out=r_En[:, :], in_=r_cw[:, :], func=AF.Exp, scale=C0), ["r_cw"], ["r_En"])
            P.op("act", lambda e: e.activation(out=r_Ea[:, :], in_=r_cx[:, :], func=AF.Exp, scale=-C0), ["r_cx"], ["r_Ea"])
            P.op("act", lambda e: e.activation(out=r_El[:, :], in_=r_dl[:, :], func=AF.Exp, scale=-C0), ["r_dl"], ["r_El"])
            r_dg = RDGL[blk % 2]
            P.op("pool", lambda e: e.tensor_copy(out=r_dg[:, :], in_=v3(r_Er)[:, :, 63]), ["r_Er"], [kRdg])
            P.op("dve", lambda e: e.scalar_tensor_tensor(out=FM[:, :, 0, :], in0=v3(r_kkn), scalar=-1.0, in1=v3(r_Ea),
                                                         op0=ALU.mult, op1=ALU.mult), ["r_kkn", "r_Ea"], ["FM"])
            P.op("dve", lambda e: e.tensor_tensor(out=r_rt32[:, :], in0=r_r[:, :], in1=r_Er[:, :], op=ALU.mult),
                 ["r_r", "r_Er"], ["r_rt32"])
            P.op("act", lambda e: e.activation(out=FM[:, :, 1, :], in_=v3(r_rt32), func=AF.Copy), ["r_rt32"], ["FM"])
            P.op("dve", lambda e: e.tensor_tensor(out=FM[:, :, 2, :], in0=v3(r_bv), in1=v3(r_En), op=ALU.mult),
                 ["r_bv", "r_En"], ["FM"])
            P.op("pool", lambda e: e.tensor_tensor(out=FM[:, :, 3, :], in0=v3(r_k2), in1=v3(r_En), op=ALU.mult),
                 ["r_k2", "r_En"], ["FM"])
            P.op("dve", lambda e: e.tensor_tensor(out=r_bh[:, :], in0=r_bv[:, :], in1=r_El[:, :], op=ALU.mult),
                 ["r_bv", "r_El"], ["r_bh"])
            P.op("pool", lambda e: e.tensor_tensor(out=r_kh[:, :], in0=r_k2[:, :], in1=r_El[:, :], op=ALU.mult),
                 ["r_k2", "r_El"], ["r_kh"])
            P.op("dve", lambda e: e.scalar_tensor_tensor(out=r_prod[:, :], in0=r_r[:, :], scalar=pvs("rk"), in1=r_k2[:, :],
                                                         op0=ALU.mult, op1=ALU.mult), ["r_r", "r_k2", "PV"], ["r_prod"])
            for cp in range(4):
                b = P.bank()
                for cc in range(2):
                    c = cp * 2 + cc
                    for i, (src, sk) in enumerate(((None, "FM"), (r_bh, "r_bh"), (r_kh, "r_kh"))):
                        in_ap = FM[:, c, 0, :] if src is None else src[:, c * 64:(c + 1) * 64]
                        P.op("pe", lambda e, in_ap=in_ap, cc=cc, i=i, b=b: e.transpose(
                            out=PBbf(b)[0:64, (cc * 3 + i) * 128:(cc * 3 + i + 1) * 128], in_=in_ap, identity=identb[:, :]),
                             [sk, "identb"], [bk(b)])
                P.op("act", lambda e, cp=cp, b=b: e.activation(
                    out=TMa[:, cp * 2:cp * 2 + 2, :],
                    in_=PBbf(b)[0:64, 0:768].rearrange("p (c i k) -> p c i k", c=2, i=3)[:, :, 0, :],
                    func=AF.Copy), [bk(b)], ["TMa"])
                P.op("dve", lambda e, cp=cp, b=b: e.tensor_copy(
                    out=TMb[:, cp * 2:cp * 2 + 2, :, :],
                    in_=PBbf(b)[0:64, 0:768].rearrange("p (c i k) -> p c i k", c=2, i=3)[:, :, 1:3, :]),
                     [bk(b)], [kTMb])
            bb = P.bank()
            for c in range(8):
                P.op("pe", lambda e, c=c: e.matmul(PB(bb)[0:64, c * 2:c * 2 + 2], lhsT=r_prod[:, c * 64:(c + 1) * 64],
                                                   rhs=cs("hsel"), start=True, stop=True), ["r_prod", "CST"], [bk(bb)])
            P.op("dve", lambda e: e.tensor_copy(out=bon[:, :, :].rearrange("p c h -> p (c h)"), in_=PB(bb)[0:64, 0:16]),
                 [bk(bb)], [kbon])
            for half in range(2):
                b = P.bank()
                for cc in range(4):
                    c = half * 4 + cc
                    P.op("pe", lambda e, c=c, cc=cc, b=b: e.matmul(PB(b)[0:64, cc * 128:(cc + 1) * 128],
                                                                   lhsT=r_sg[:, c * 64:(c + 1) * 64], rhs=lorb[0:96, 256:384],
                                                                   start=True, stop=True), ["r_sg", "Wa"], [bk(b)])
                P.op("act", lambda e, half=half, b=b: e.activation(
                    out=Gt[:, half * 4:half * 4 + 4, :].rearrange("p c k -> p (c k)"), in_=PB(b)[0:64, :], func=AF.Copy),
                     [bk(b)], [kGt])

            bq = proj_fm(0, 128, False)
            P.op("act", lambda e: e.activation(out=hq2[:, :], in_=PB(bq)[:, :], func=AF.Silu), [bk(bq)], ["hq2"])
            bf_ = proj_fm(128, 128, False)
            P.op("act", lambda e: e.activation(out=hf2[:, :], in_=PB(bf_)[:, :], func=AF.Sigmoid), [bk(bf_)], ["hf2"])
            return None
        r_dg = RDGL[blk % 2]
        h_dg = HDGL[blk % 2]
        P.begin_capture([4])
        chk("A2")
        P.op("dve", lambda e: e.tensor_scalar(out=h_f[:, :], in0=hf2[:, :], scalar1=OML, scalar2=LB,
                                              op0=ALU.mult, op1=ALU.add), ["hf2", "lbv"], ["h_f"])
        chk("H2")
        P.op("act", lambda e: e.activation(out=h_lf[:, :], in_=h_f[:, :], func=AF.Ln), ["h_f"], ["h_lf"])
        P.op("pool", lambda e: e.tensor_scalar(out=h_kk[:, :], in0=h_f[:, :], scalar1=-1.0, scalar2=1.0,
                                               op0=ALU.mult, op1=ALU.add), ["h_f"], ["h_kk"])
        chk("H3")
        P.op("dve", lambda e: e.tensor_tensor_scan(out=h_cum[:, :], data0=cs("rmask"), data1=h_lf[:, :], initial=0.0,
                                                   op0=ALU.mult, op1=ALU.add), ["h_lf", "CST"], ["h_cum"])
        chk("H4")
        P.op("act", lambda e: e.activation(out=h_e[:, :], in_=h_cum[:, :], func=AF.Exp), ["h_cum"], ["h_e"])
        P.op("dve", lambda e: e.tensor_tensor(out=h_qhat[:, :], in0=hq2[:, :], in1=h_e[:, :], op=ALU.mult),
             ["hq2", "h_e"], ["h_qhat"])
        chk("H5")
        h_dg = HDGL[blk % 2]
        P.op("pool", lambda e: e.tensor_copy(out=h_dg[:, :], in_=v3(h_e)[:, :, 63]), ["h_e"], [kHdg])
        chk("H6")
        P.op("dve", lambda e: e.tensor_tensor(out=v3(h_t), in0=v3(h_cum), in1=v3(h_cum)[:, :, 31:32].to_broadcast([128, 8, 64]),
                                              op=ALU.subtract), ["h_cum"], ["h_t"])
        P.op("act", lambda e: e.activation(out=h_e[:, :], in_=h_t[:, :], func=AF.Exp), ["h_t", kHdg], ["h_e"])
        P.op("dve", lambda e: e.tensor_tensor(out=h_qtl[:, :], in0=hq2[:, :], in1=h_e[:, :], op=ALU.mult),
             ["hq2", "h_e"], ["h_qtl"])
        P.op("act", lambda e: e.activation(out=h_e[:, :], in_=h_t[:, :], func=AF.Exp, scale=-1.0), ["h_t", "h_qtl"], ["h_e"])
        P.op("dve", lambda e: e.tensor_tensor(out=h_ktl[:, :], in0=h_kk[:, :], in1=h_e[:, :], op=ALU.mult),
             ["h_kk", "h_e"], ["h_ktl"])
        chk("H7")
        P.op("dve", lambda e: e.tensor_tensor(out=v3(h_t), in0=v3(h_cum)[:, :, 63:64].to_broadcast([128, 8, 64]),
                                              in1=v3(h_cum), op=ALU.subtract), ["h_cum", "h_e"], ["h_t"])
        P.op("act", lambda e: e.activation(out=h_e[:, :], in_=h_t[:, :], func=AF.Exp), ["h_t", "h_ktl"], ["h_e"])
        P.op("dve", lambda e: e.tensor_tensor(out=h_khat[:, :], in0=h_kk[:, :], in1=h_e[:, :], op=ALU.mult),
             ["h_kk", "h_e"], ["h_khat"])
        chk("H8")
        bt_ = P.bank()
        for c in range(8):
            P.op("pe", lambda e, c=c: e.transpose(out=PBbf(bt_)[0:64, c * 128:(c + 1) * 128],
                                                  in_=h_khat[:, c * 64:(c + 1) * 64], identity=identb[:, :]),
                 ["h_khat", "identb"], [bk(bt_)])
        P.op("act", lambda e: e.activation(out=h_khT[:, :, :].rearrange("p c k -> p (c k)"), in_=PBbf(bt_)[0:64, :],
                                           func=AF.Copy), [bk(bt_)], ["h_khT"])
        chk("H9")
        bs_ = P.bank()
        for c in range(8):
            P.op("pe", lambda e, c=c: e.matmul(PB(bs_)[0:64, c * 64:(c + 1) * 64], lhsT=h_ktl[:, c * 64:(c + 1) * 64],
                                               rhs=h_qtl[:, c * 64:(c + 1) * 64], start=True, stop=True),
                 ["h_ktl", "h_qtl"], [bk(bs_)])
        chk("H10")
        P.op("dve", lambda e: e.tensor_tensor(out=h_scT[:, :, :].rearrange("p c t -> p (c t)"), in0=cs("hmask", 64),
                                              in1=PB(bs_)[0:64, :], op=ALU.mult), [bk(bs_), "CST"], ["h_scT"])
        capH = P.end_capture()
        P.begin_capture([0, 1, 2, 3])
        chk("A4")
        ob_banks = [6, 7]
        for pg in range(4):
            c0 = pg * 2
            Nm, nmk = NmL[pg], "Nm%d" % pg
            Z32, z32k, Zbf, zbk = Z32L[pg], "Z32%d" % pg, ZbfL[pg], "Zbf%d" % pg
            Akp, Nkp = AkpL[pg], NkpL[pg]
            for cc in range(2):
                c = c0 + cc
                b = P.bank()
                for h in range(2):
                    ph = slice(h * 64, (h + 1) * 64)
                    rhs = FM[ph, c, 0:2, :].rearrange("p a t -> p (a t)")
                    P.op("pe", lambda e, ph=ph, c=c, h=h, b=b, rhs=rhs: e.matmul(
                        PB(b)[0:64, h * 256:h * 256 + 128], lhsT=FM[ph, c, 2, :], rhs=rhs, start=True, stop=True),
                         ["FM"], [bk(b)], rg=h * 64)
                    P.op("pe", lambda e, ph=ph, c=c, h=h, b=b, rhs=rhs: e.matmul(
                        PB(b)[0:64, h * 256 + 128:h * 256 + 256], lhsT=FM[ph, c, 3, :], rhs=rhs, start=True, stop=True),
                         ["FM"], [bk(b)], rg=h * 64)
                P.op("dve", lambda e, cc=cc, b=b, Nm=Nm: e.tensor_tensor(
                    out=Nm[:, cc * 2:cc * 2 + 2, :, :].rearrange("p u i t -> p (u i t)"), in0=cs("nmask", 64),
                    in1=PB(b)[0:64, :], op=ALU.mult), [bk(b), "CST"], [nmk])
            b = P.bank()
            for h in range(2):
                for cc in range(2):
                    u = cc * 2 + h
                    c = c0 + cc
                    ph = slice(h * 64, (h + 1) * 64)
                    P.op("pe", lambda e, u=u, c=c, ph=ph, b=b: e.matmul(PB(b)[0:64, u * 64:(u + 1) * 64], lhsT=FM[ph, c, 0, :],
                                                                        rhs=FM[ph, c, 2, :], start=True, stop=True),
                         ["FM"], [bk(b)], rg=h * 64)
            for u in range(4):
                cc, h = u // 2, u % 2
                c = c0 + cc
                P.op("pe", lambda e, u=u, c=c, h=h, b=b, Nm=Nm: e.matmul(PB(b)[0:64, 256 + u * 64:256 + (u + 1) * 64],
                                                                         lhsT=Nm[:, u, 2, :], rhs=Vr[:, c, h * 64:(h + 1) * 64],
                                                                         start=True, stop=True), [nmk, kVr], [bk(b)])
            P.op("dve", lambda e, b=b, Akp=Akp: e.tensor_tensor(out=Akp[0][:, :, :].rearrange("p u s -> p (u s)"),
                                                                in0=cs("amask", 64), in1=PB(b)[0:64, 0:256], op=ALU.mult),
                 [bk(b), "CST"], ["Ak0_%d" % pg])
            P.op("act", lambda e, b=b, Z32=Z32: e.activation(out=Z32[:, :, 64:128],
                                                             in_=PB(b)[0:64, 256:512].rearrange("p (u v) -> p u v", v=64),
                                                             func=AF.Copy), [bk(b)], [z32k])
            P.op("pool", lambda e, c0=c0, Z32=Z32: e.tensor_copy(out=Z32[:, :, 0:64].rearrange("p (c h) k -> p c h k", h=2),
                                                                 in_=TMa[:, c0:c0 + 2, :].rearrange("p c (h k) -> p c h k", k=64)),
                 ["TMa"], [z32k])
            P.op("pool", lambda e, Nkp=Nkp, Nm=Nm: e.tensor_copy(out=Nkp[0][:, :, :], in_=Nm[:, :, 0, :]), [nmk], ["Nk0_%d" % pg])
            P.op("act", lambda e, Zbf=Zbf, Z32=Z32: e.activation(out=Zbf[:, :, :], in_=Z32[:, :, :], func=AF.Copy), [z32k], [zbk])
        for lvl in range(6):
            for pg in range(4):
                Z32, z32k, Zbf, zbk = Z32L[pg], "Z32%d" % pg, ZbfL[pg], "Zbf%d" % pg
                pi = lvl % 2
                Ak, Nk = AkpL[pg][pi], NkpL[pg][pi]
                akk, nkk = "Ak%d_%d" % (pi, pg), "Nk%d_%d" % (pi, pg)
                b = P.bank()
                for u in range(4):
                    P.op("pe", lambda e, u=u, b=b, Nk=Nk, Zbf=Zbf: e.matmul(PB(b)[0:64, u * 128:(u + 1) * 128], lhsT=Nk[:, u, :],
                                                                            rhs=Zbf[:, u, :], start=True, stop=True),
                         [nkk, zbk], [bk(b)])
                if lvl < 5:
                    b2 = P.bank()
                    for u in range(4):
                        P.op("pe", lambda e, u=u, b2=b2, Nk=Nk, Ak=Ak: e.matmul(
                            PB(b2)[0:64, u * 64:(u + 1) * 64], lhsT=Ak[:, u, :], rhs=Nk[:, u, :], start=True, stop=True),
                             [akk, nkk], [bk(b2)])
                        P.op("pe", lambda e, u=u, b2=b2, Nk=Nk, Ak=Ak: e.matmul(
                            PB(b2)[0:64, 256 + u * 64:256 + (u + 1) * 64], lhsT=Nk[:, u, :], rhs=Ak[:, u, :],
                            start=True, stop=True), [akk, nkk], [bk(b2)])
                P.op("dve", lambda e, b=b, Z32=Z32: e.tensor_tensor(out=Z32[:, :, :].rearrange("p u k -> p (u k)"),
                                                                    in0=Z32[:, :, :].rearrange("p u k -> p (u k)"),
                                                                    in1=PB(b)[0:64, :], op=ALU.add), [bk(b), z32k], [z32k])
                P.op("act", lambda e, Zbf=Zbf, Z32=Z32: e.activation(out=Zbf[:, :, :], in_=Z32[:, :, :], func=AF.Copy), [z32k], [zbk])
                if lvl < 5:
                    P.op("act", lambda e, b2=b2, pi=pi, pg=pg: e.activation(
                        out=NkpL[pg][1 - pi][:, :, :].rearrange("p u s -> p (u s)"), in_=PB(b2)[0:64, 0:256], func=AF.Copy),
                         [bk(b2)], ["Nk%d_%d" % (1 - pi, pg)])
                    P.op("dve", lambda e, b2=b2, pi=pi, pg=pg: e.tensor_copy(
                        out=AkpL[pg][1 - pi][:, :, :].rearrange("p u s -> p (u s)"), in_=PB(b2)[0:64, 256:512]),
                         [bk(b2)], ["Ak%d_%d" % (1 - pi, pg)])
        for pg in range(4):
            c0 = pg * 2
            Nm, nmk = NmL[pg], "Nm%d" % pg
            Zbf, zbk = ZbfL[pg], "Zbf%d" % pg
            RhT, MT = RhTL[pg], MTL[pg]
            b = P.bank()
            for u in range(4):
                cc, h = u // 2, u % 2
                c = c0 + cc
                P.op("pe", lambda e, u=u, cc=cc, h=h, b=b, Zbf=Zbf, Nm=Nm: e.matmul(
                    PB(b)[h * 64:(h + 1) * 64, cc * 64:(cc + 1) * 64], lhsT=Zbf[:, u, 0:64], rhs=Nm[:, u, 1, :],
                    start=True, stop=True), [zbk, nmk], [bk(b)])
                P.op("pe", lambda e, u=u, cc=cc, h=h, c=c, b=b, Zbf=Zbf: e.matmul(
                    PB(b)[h * 64:(h + 1) * 64, 128 + cc * 64:128 + (cc + 1) * 64], lhsT=Zbf[:, u, 0:64],
                    rhs=TMb[:, c, 0, h * 64:(h + 1) * 64], start=True, stop=True), [zbk, kTMb], [bk(b)])
            P.op("dve", lambda e, b=b, c0=c0, RhT=RhT: e.tensor_tensor(out=RhT[:, :, :], in0=v3(r_rt32)[:, c0:c0 + 2, :],
                                                                       in1=PB(b)[:, 0:128].rearrange("p (c t) -> p c t", t=64), op=ALU.add),
                 [bk(b), "r_rt32"], ["RhT%d" % pg])
            P.op("act", lambda e, b=b, MT=MT: e.activation(out=MT[:, :, :].rearrange("p c k -> p (c k)"), in_=PB(b)[:, 128:256],
                                                           func=AF.Copy), [bk(b)], ["MT%d" % pg])
        capW = P.end_capture()
        P.replay_merged([capH, capW] + ([EXTRA["cap"]] if EXTRA["cap"] else []))
        EXTRA["cap"] = None
        return None


    import os
    KSTOP = os.environ.get("KSTOP", "")
    if KSTOP == "A0":
        P.emit([])
        return nc
    def issue_cc(blk):
        q = ((blk + 1) * 512 - 1) // NT
        if (blk + 1) * 512 % NT == 0 or NT < 512:
            qs = [q] if NT >= 512 else list(range(4))
            for qq in qs:
                if os.environ.get("KSKIP_CC"):
                    P.dma("pool", xout[qq][:, :], xin[qq][:, :], "ccfake", reads=["xin%d" % qq], writes=["xout%d" % qq])
                    continue
                P.op("pool", lambda e, qq=qq: e.collective_compute(
                    "AllReduce", ALU.add, replica_groups=[[0, 1, 2, 3], [4, 5, 6, 7]],
                    ins=[xin[qq].ap().opt()], outs=[xout[qq].ap().opt()]),
                     reads=["xin%d" % qq], writes=["xout%d" % qq], sem="CC", inc=1)

    try:
        P.begin_capture([0, 1, 2, 3])
        phaseA_block(0, "norm")
        phaseA_block(0, "front2")
        P.replay_merged([P.end_capture()])
        for blk in range(NB):
            if blk + 1 < NB:
                P.begin_capture([5])
                phaseA_block(blk + 1, "norm")
                EXTRA["cap"] = P.end_capture()
            phaseA_block(blk, "rest")
            if os.environ.get("KNOP4MERGE"):
                phaseA_p4(blk)
                P.begin_capture([4, 5])
            else:
                P.begin_capture([4, 5])
                phaseA_p4(blk)
            phaseA_out(blk)
            caps = [P.end_capture()]
            if blk + 1 < NB:
                P.begin_capture([0, 1, 2])
                phaseA_block(blk + 1, "front2")
                caps.append(P.end_capture())
            P.replay_merged(caps)
            chk("A7")
            issue_cc(blk)
    except _Stop:
        P.emit([])
        return nc
    if KSTOP == "A":
        P.emit([])
        return nc
    P.barrier()
    stA.close()
    P.nb = 8

    stB = contextlib.ExitStack()
    cur[0] = stP
    h2T = sb([128, 8, NT], BF16)
    Wt = sb([128, NTT, 32])
    gt1b = sb([128, D])
    gt2b = sb([128, D])
    fgb_t = sb([128, D])
    cur[0] = stB
    fgb = din("fgb", [128, D])
    xown = din("xown", [NT, D])
    P.dma("sp", fgb_t[:, :], fgb[:, :], "fgb", writes=["fgb"])
    dgt = [sb([128, 128]) for _ in range(2)]
    di = [0]

    def row_bcast(vec, out_t, outk):
        for half in range(2):
            b = P.bank()
            for k4 in range(4):
                kc = half * 4 + k4
                d_, dk = dgt[di[0] % 2], "dgt%d" % (di[0] % 2)
                di[0] += 1
                P.op("dve", lambda e, d_=d_, kc=kc: e.tensor_scalar(out=d_[:, :], in0=cs("ident"), scalar1=vec[:, kc:kc + 1],
                                                                    scalar2=None, op0=ALU.mult), ["CST", "mod"], [dk])
                P.op("pe", lambda e, d_=d_, k4=k4, b=b: e.matmul(PB(b)[:, k4 * 128:(k4 + 1) * 128], lhsT=cs("ones"), rhs=d_[:, :],
                                                                 start=True, stop=True), ["CST", dk], [bk(b)])
            P.op("act", lambda e, half=half, b=b: e.activation(out=out_t[:, half * 512:(half + 1) * 512], in_=PB(b)[:, :],
                                                               func=AF.Copy), [bk(b)], [outk])

    row_bcast(mod[:, 16:24], gt1b, "gt1b")
    row_bcast(mod[:, 40:48], gt2b, "gt2b")

    NF["junk"] = sb([128, D], BF16)
    NF["xn"] = [sb([128, D], BF16) for _ in range(2)]
    NF["stat"] = [sb([128, 4]) for _ in range(2)]
    junk, stat = NF["junk"], NF["stat"]
    stgB = [sb([128, 8, 256]) for _ in range(2)]
    sbi = [0]

    def load_castB(dst_fn, src_ap, nk, ncols, wk):
        t, k = stgB[sbi[0] % 2], "stgB%d" % (sbi[0] % 2)
        eng = "sp" if sbi[0] % 2 == 0 else "act"
        sbi[0] += 1
        P.dma(eng, t[:, 0:nk, 0:ncols], src_ap.rearrange("(kc p) n -> p kc n", p=128), k, writes=[k])
        for kc in range(nk):
            if kc % 2:
                P.op("act", lambda e, kc=kc: e.activation(out=dst_fn(kc), in_=t[:, kc, 0:ncols], func=AF.Copy), [k], [wk])
            else:
                P.op("dve", lambda e, kc=kc: e.tensor_copy(out=dst_fn(kc), in_=t[:, kc, 0:ncols]), [k], [wk])

    Wgt = sb([128, 8, 2048], BF16)
    Wpa = sb([128, 4, D], BF16)
    Wpb = sb([128, 4, D], BF16)
    Wo = sb([128, 8, D], BF16)
    Wr32 = sb([128, 8, 36])
    for c4 in range(8):
        load_castB(lambda kc, c4=c4: Wgt[:, kc, c4 * 256:(c4 + 1) * 256], wgates[:, c4 * 256:(c4 + 1) * 256], 8, 256, "WB")
    for c2 in range(4):
        load_castB(lambda kc, c2=c2: Wpa[:, kc, c2 * 256:(c2 + 1) * 256], wpa[:, c2 * 256:(c2 + 1) * 256], 4, 256, "WB")
        load_castB(lambda kc, c2=c2: Wpb[:, kc, c2 * 256:(c2 + 1) * 256], wpb[:, c2 * 256:(c2 + 1) * 256], 4, 256, "WB")
        load_castB(lambda kc, c2=c2: Wo[:, kc, c2 * 256:(c2 + 1) * 256], wout[:, c2 * 256:(c2 + 1) * 256], 8, 256, "WB")
    P.dma("sp", Wr32[:, :, :], wr[:, :].rearrange("(kc p) n -> p kc n", p=128), "wr", writes=["WB"])

    TB = min(256, NT)
    TBT = TB // 128
    xB = sb([128, TBT, D])
    hB = sb([128, 8, TB], BF16)
    oT = sb([128, 8, TB], BF16)
    mixT = sb([128, 8, TB], BF16)
    xo = [sb([128, D], BF16) for _ in range(4)]
    oc = sb([128, D], BF16)
    gsa = sb([128, TB])
    gsb = sb([128, TB])
    m1t = sb([128, TB])
    m2t = sb([128, TB])
    x1t = [sb([128, D]) for _ in range(2)]
    x1n = sb([128, D])
    h32 = sb([128, 8, 128])
    rt_ = sb([128, 64])
    L = sb([128, 36])
    lem = sb([128, 32])
    lem2 = sb([128, 32])
    mk1 = sb([128, 32])
    mk2 = sb([128, 32])
    identf = cs("ident")

    for blk in range(NT // TB):
        for tt in range(TBT):
            r0 = blk * TB + tt * 128
            P.dma("sp", xB[:, tt, :], xown[r0:r0 + 128, :], "xB", writes=["xB"])
            i = nti[0] % 2
            norm_fm(xB[:, tt, :], "xB", s1, mod, "s1", lambda kc, tt=tt: hB[:, kc, tt * 128:(tt + 1) * 128], "hB")
            for qq in range(4):
                P.dma("act" if qq % 2 else "sp", xo[qq][:, :], xout[qq][r0:r0 + 128, :], "xo%d" % qq,
                      reads=["xout%d" % qq], writes=["xo%d" % qq])
            P.op("dve", lambda e: e.tensor_scalar(out=oc[:, :], in0=xo[0][:, :], scalar1=pvs("sel")[:, 0:1], scalar2=None,
                                                  op0=ALU.mult), ["xo0", "PV"], ["oc"])
            for qq in range(1, 4):
                P.op("dve", lambda e, qq=qq: e.scalar_tensor_tensor(out=oc[:, :], in0=xo[qq][:, :], scalar=pvs("sel")[:, qq:qq + 1],
                                                                    in1=oc[:, :], op0=ALU.mult, op1=ALU.add),
                     ["xo%d" % qq, "oc", "PV"], ["oc"])
            b = P.bank()
            for ch in range(8):
                P.op("pe", lambda e, ch=ch, b=b: e.transpose(out=PBbf(b)[:, ch * 128:(ch + 1) * 128],
                                                             in_=oc[:, ch * 128:(ch + 1) * 128], identity=identb[:, :]),
                     ["oc", "identb"], [bk(b)])
            P.op("act", lambda e, tt=tt, b=b: e.activation(out=oT[:, :, tt * 128:(tt + 1) * 128],
                                                           in_=PBbf(b)[:, :].rearrange("p (c t) -> p c t", t=128), func=AF.Copy),
                 [bk(b)], ["oT"])
        for mc in range(8):
            ba, bb_, bga, bgb = P.bank(), P.bank(), P.bank(), P.bank()
            for j_ in range(4):
                P.op("pe", lambda e, j_=j_, mc=mc, ba=ba: e.matmul(PB(ba)[:, 0:TB], lhsT=Wpa[:, j_, mc * 128:(mc + 1) * 128],
                                                                   rhs=oT[:, j_ * 2, :], start=(j_ == 0), stop=(j_ == 3)),
                     ["WB", "oT"], [bk(ba)])
            for j_ in range(4):
                P.op("pe", lambda e, j_=j_, mc=mc, bb_=bb_: e.matmul(PB(bb_)[:, 0:TB], lhsT=Wpb[:, j_, mc * 128:(mc + 1) * 128],
                                                                     rhs=oT[:, j_ * 2 + 1, :], start=(j_ == 0), stop=(j_ == 3)),
                     ["WB", "oT"], [bk(bb_)])
            for kc in range(8):
                P.op("pe", lambda e, kc=kc, mc=mc, bga=bga: e.matmul(PB(bga)[:, 0:TB], lhsT=Wgt[:, kc, mc * 128:(mc + 1) * 128],
                                                                     rhs=hB[:, kc, :], start=(kc == 0), stop=(kc == 7)),
                     ["WB", "hB"], [bk(bga)])
            for kc in range(8):
                P.op("pe", lambda e, kc=kc, mc=mc, bgb=bgb: e.matmul(PB(bgb)[:, 0:TB], lhsT=Wgt[:, kc, D + mc * 128:D + (mc + 1) * 128],
                                                                     rhs=hB[:, kc, :], start=(kc == 0), stop=(kc == 7)),
                     ["WB", "hB"], [bk(bgb)])
            P.op("act", lambda e, bga=bga: e.activation(out=gsa[:, :], in_=PB(bga)[:, 0:TB], func=AF.Sigmoid), [bk(bga)], ["gsa"])
            P.op("act", lambda e, bgb=bgb: e.activation(out=gsb[:, :], in_=PB(bgb)[:, 0:TB], func=AF.Sigmoid), [bk(bgb)], ["gsb"])
            P.op("dve", lambda e, ba=ba: e.tensor_tensor(out=m1t[:, :], in0=gsa[:, :], in1=PB(ba)[:, 0:TB], op=ALU.mult),
                 ["gsa", bk(ba)], ["m1t"])
            P.op("dve", lambda e, bb_=bb_: e.tensor_tensor(out=m2t[:, :], in0=gsb[:, :], in1=PB(bb_)[:, 0:TB], op=ALU.mult),
                 ["gsb", bk(bb_)], ["m2t"])
            P.op("pool", lambda e, mc=mc: e.tensor_tensor(out=mixT[:, mc, :], in0=m1t[:, :], in1=m2t[:, :], op=ALU.add),
                 ["m1t", "m2t"], ["mixT"])
        for tt in range(TBT):
            tile_i = blk * TBT + tt
            r0 = tile_i * 128
            x1, x1k = x1t[tile_i % 2], "x1t%d" % (tile_i % 2)
            for ch in range(2):
                b = P.bank()
                for kc in range(8):
                    P.op("pe", lambda e, kc=kc, tt=tt, ch=ch, b=b: e.matmul(PB(b)[:, :], lhsT=mixT[:, kc, tt * 128:(tt + 1) * 128],
                                                                            rhs=Wo[:, kc, ch * 512:(ch + 1) * 512],
                                                                            start=(kc == 0), stop=(kc == 7)), ["mixT", "WB"], [bk(b)])
                P.op("dve", lambda e, ch=ch, b=b, x1=x1: e.tensor_tensor(out=x1[:, ch * 512:(ch + 1) * 512], in0=PB(b)[:, :],
                                                                         in1=gt1b[:, ch * 512:(ch + 1) * 512], op=ALU.mult),
                     [bk(b), "gt1b"], [x1k])
            P.op("pool", lambda e, tt=tt, x1=x1: e.tensor_tensor(out=x1[:, :], in0=x1[:, :], in1=xB[:, tt, :], op=ALU.add),
                 [x1k, "xB"], [x1k])
            P.dma("act", x1s[r0:r0 + 128, :], x1[:, :], "x1st%d" % (tile_i % 2), reads=[x1k], writes=["x1s"])
            st_, sk = stat[tile_i % 2], "stat%d" % (tile_i % 2)
            P.op("act", lambda e, x1=x1, st_=st_: e.activation(out=junk[:, :], in_=x1[:, :], func=AF.Square, accum_out=st_[:, 0:1]),
                 [x1k], ["junk", sk])
            P.op("dve", lambda e, st_=st_: e.tensor_scalar(out=st_[:, 1:2], in0=st_[:, 0:1], scalar1=1.0 / D, scalar2=1e-6,
                                                           op0=ALU.mult, op1=ALU.add), [sk], [sk])
            rsq(st_[:, 2:3], st_[:, 1:2], [sk], [sk])
            P.op("dve", lambda e, x1=x1, st_=st_: e.tensor_scalar(out=x1n[:, :], in0=x1[:, :], scalar1=st_[:, 2:3], scalar2=None,
                                                                  op0=ALU.mult), [x1k, sk], ["x1n"])
            for half in range(2):
                b = P.bank()
                for k4 in range(4):
                    kc = half * 4 + k4
                    P.op("pe", lambda e, kc=kc, k4=k4, b=b: e.transpose(out=PB(b)[:, k4 * 128:(k4 + 1) * 128],
                                                                        in_=x1n[:, kc * 128:(kc + 1) * 128], identity=identf),
                         ["x1n", "CST"], [bk(b)])
                for k4 in range(4):
                    kc = half * 4 + k4
                    P.op("act", lambda e, kc=kc, k4=k4, b=b: e.activation(out=h32[:, kc, :], in_=PB(b)[:, k4 * 128:(k4 + 1) * 128],
                                                                          func=AF.Identity, scale=s2[:, kc:kc + 1],
                                                                          bias=mod[:, 24 + kc:25 + kc]), [bk(b), "s2", "mod"], ["h32"])
            P.op("pool", lambda e, r0=r0: e.tensor_copy(out=h2T[:, :, r0:r0 + 128], in_=h32[:, :, :]), ["h32"], ["h2T"])
            b = P.bank()
            for kc in range(8):
                P.op("pe", lambda e, kc=kc, b=b: e.matmul(PB(b)[:, 0:36], lhsT=h32[:, kc, :], rhs=Wr32[:, kc, :],
                                                          start=(kc == 0), stop=(kc == 7)), ["h32", "WB"], [bk(b)])
            R = lambda a, b_: rt_[:, a:b_]
            V = lambda fn, rd, wr_: P.op("dve", fn, rd, wr_)
            V(lambda e, b=b: e.tensor_tensor(out=L[:, :], in0=PB(b)[:, 0:36], in1=pvs("rb"), op=ALU.add), [bk(b), "PV"], ["L"])
            V(lambda e: e.tensor_reduce(out=R(0, 1), in_=L[:, 0:4], axis=AX.X, op=ALU.max), ["L"], ["rt"])
            V(lambda e: e.tensor_scalar(out=R(1, 2), in0=R(0, 1), scalar1=-1.0, scalar2=None, op0=ALU.mult), ["rt"], ["rt"])
            P.op("act", lambda e: e.activation(out=R(4, 8), in_=L[:, 0:4], func=AF.Exp, bias=R(1, 2), accum_out=R(2, 3)),
                 ["L", "rt"], ["rt"])
            V(lambda e: e.reciprocal(out=R(3, 4), in_=R(2, 3)), ["rt"], ["rt"])
            V(lambda e: e.tensor_scalar(out=R(8, 12), in0=L[:, 0:4], scalar1=R(0, 1), scalar2=None, op0=ALU.is_equal),
              ["L", "rt"], ["rt"])
            V(lambda e: e.tensor_scalar(out=R(12, 16), in0=R(8, 12), scalar1=-1.0, scalar2=1e30, op0=ALU.add, op1=ALU.mult),
              ["rt"], ["rt"])
            V(lambda e: e.tensor_tensor(out=lem[:, :].rearrange("p (g k) -> p g k", k=8),
                                        in0=L[:, 4:36].rearrange("p (g k) -> p g k", k=8),
                                        in1=R(12, 16).unsqueeze(2).to_broadcast([128, 4, 8]), op=ALU.add), ["L", "rt"], ["lem"])
            V(lambda e: e.tensor_reduce(out=R(16, 17), in_=lem[:, :], axis=AX.X, op=ALU.max), ["lem"], ["rt"])
            V(lambda e: e.tensor_scalar(out=mk1[:, :], in0=lem[:, :], scalar1=R(16, 17), scalar2=None, op0=ALU.is_equal),
              ["lem", "rt"], ["mk1"])
            V(lambda e: e.scalar_tensor_tensor(out=lem2[:, :], in0=mk1[:, :], scalar=-1e30, in1=lem[:, :], op0=ALU.mult, op1=ALU.add),
              ["mk1", "lem"], ["lem2"])
            V(lambda e: e.tensor_reduce(out=R(17, 18), in_=lem2[:, :], axis=AX.X, op=ALU.max), ["lem2"], ["rt"])
            V(lambda e: e.tensor_scalar(out=mk2[:, :], in0=lem2[:, :], scalar1=R(17, 18), scalar2=None, op0=ALU.is_equal),
              ["lem2", "rt"], ["mk2"])
            V(lambda e: e.tensor_scalar(out=R(18, 19), in0=R(16, 17), scalar1=-1.0, scalar2=None, op0=ALU.mult), ["rt"], ["rt"])
            P.op("act", lambda e: e.activation(out=R(19, 20), in_=R(17, 18), func=AF.Exp, bias=R(18, 19)), ["rt"], ["rt"])
            V(lambda e: e.tensor_scalar(out=R(20, 21), in0=R(19, 20), scalar1=1.0, scalar2=None, op0=ALU.add), ["rt"], ["rt"])
            V(lambda e: e.reciprocal(out=R(21, 22), in_=R(20, 21)), ["rt"], ["rt"])
            V(lambda e: e.tensor_tensor(out=R(22, 23), in0=R(21, 22), in1=R(3, 4), op=ALU.mult), ["rt"], ["rt"])
            V(lambda e: e.tensor_tensor(out=R(23, 24), in0=R(22, 23), in1=R(19, 20), op=ALU.mult), ["rt"], ["rt"])
            V(lambda e: e.tensor_scalar(out=mk1[:, :], in0=mk1[:, :], scalar1=R(22, 23), scalar2=None, op0=ALU.mult),
              ["mk1", "rt"], ["mk1"])
            V(lambda e, tile_i=tile_i: e.scalar_tensor_tensor(out=Wt[:, tile_i, :], in0=mk2[:, :], scalar=R(23, 24), in1=mk1[:, :],
                                                              op0=ALU.mult, op1=ALU.add), ["mk2", "mk1", "rt"], ["Wt"])
    P.barrier()
    stB.close()

    stM = contextlib.ExitStack()
    cur[0] = stM
    TGT = TG // 128
    acc = sb([128, TGT, D])
    stgM = [sb([128, 8, 256]) for _ in range(2)]
    wG = [sb([128, 8, 512], BF16) for _ in range(2)]
    wU = [sb([128, 8, 512], BF16) for _ in range(2)]
    wD = [sb([128, 4, D], BF16) for _ in range(2)]
    sgl = [sb([128, 512]) for _ in range(2)]
    hid = [sb([128, 4, 512], BF16) for _ in range(2)]
    x1r = [sb([128, D]) for _ in range(2)]
    x2 = [sb([128, D]) for _ in range(2)]
    yo = [sb([128, D]) for _ in range(2)]
    junk2 = sb([128, D], BF16)
    stat2 = [sb([128, 4]) for _ in range(2)]
    mi = [0]
    TBm = min(512, TG)

    def load_castM(dst, dk, src_ap, nk, ncols_total):
        for c0_ in range(0, ncols_total, 256):
            t, k = stgM[mi[0] % 2], "stgM%d" % (mi[0] % 2)
            eng = "sp" if mi[0] % 2 == 0 else "act"
            mi[0] += 1
            P.dma(eng, t[:, 0:nk, :], src_ap[:, c0_:c0_ + 256].rearrange("(kc p) n -> p kc n", p=128), k, writes=[k])
            P.op("act", lambda e, t=t, c0_=c0_: e.activation(out=dst[:, 0:nk // 2, c0_:c0_ + 256], in_=t[:, 0:nk // 2, :], func=AF.Copy),
                 [k], [dk])
            P.op("pool", lambda e, t=t, c0_=c0_: e.tensor_copy(out=dst[:, nk // 2:nk, c0_:c0_ + 256], in_=t[:, nk // 2:nk, :]), [k], [dk])

    hcount = [0]
    for gi in range(NG):
        g0 = gi * TG
        P.op("pool", lambda e: e.memset(acc[:, :, :], 0.0), [], ["acc"])
        for ex in range(32):
            wi = ex % 2
            load_castM(wG[wi], "wG%d" % wi, eg[ex], 8, 512)
            load_castM(wU[wi], "wU%d" % wi, eu[ex], 8, 512)
            load_castM(wD[wi], "wD%d" % wi, ed[ex], 4, D)
            for bl in range(TG // TBm):
                t0 = g0 + bl * TBm
                hd, hdk = hid[hcount[0] % 2], "hid%d" % (hcount[0] % 2)
                hcount[0] += 1
                for fc in range(4):
                    bg, bu = P.bank(), P.bank()
                    for kc in range(8):
                        P.op("pe", lambda e, kc=kc, fc=fc, bg=bg, wi=wi, t0=t0: e.matmul(
                            PB(bg)[:, 0:TBm], lhsT=wG[wi][:, kc, fc * 128:(fc + 1) * 128], rhs=h2T[:, kc, t0:t0 + TBm],
                            start=(kc == 0), stop=(kc == 7)), ["wG%d" % wi, "h2T"], [bk(bg)])
                    for kc in range(8):
                        P.op("pe", lambda e, kc=kc, fc=fc, bu=bu, wi=wi, t0=t0: e.matmul(
                            PB(bu)[:, 0:TBm], lhsT=wU[wi][:, kc, fc * 128:(fc + 1) * 128], rhs=h2T[:, kc, t0:t0 + TBm],
                            start=(kc == 0), stop=(kc == 7)), ["wU%d" % wi, "h2T"], [bk(bu)])
                    sg_, sgk = sgl[fc % 2], "sgl%d" % (fc % 2)
                    P.op("act", lambda e, bg=bg, sg_=sg_: e.activation(out=sg_[:, 0:TBm], in_=PB(bg)[:, 0:TBm], func=AF.Silu),
                         [bk(bg)], [sgk])
                    P.op("dve", lambda e, bu=bu, sg_=sg_, hd=hd, fc=fc: e.tensor_tensor(out=hd[:, fc, 0:TBm], in0=sg_[:, 0:TBm],
                                                                                        in1=PB(bu)[:, 0:TBm], op=ALU.mult),
                         [sgk, bk(bu)], [hdk])
                for tt in range(TBm // 128):
                    lt_ = bl * (TBm // 128) + tt
                    gt_ = gi * TGT + lt_
                    for ch in range(2):
                        b = P.bank()
                        for fc in range(4):
                            P.op("pe", lambda e, fc=fc, tt=tt, ch=ch, b=b, hd=hd, wi=wi: e.matmul(
                                PB(b)[:, :], lhsT=hd[:, fc, tt * 128:(tt + 1) * 128], rhs=wD[wi][:, fc, ch * 512:(ch + 1) * 512],
                                start=(fc == 0), stop=(fc == 3)), [hdk, "wD%d" % wi], [bk(b)])
                        P.op("dve", lambda e, b=b, lt_=lt_, gt_=gt_, ch=ch, ex=ex: e.scalar_tensor_tensor(
                            out=acc[:, lt_, ch * 512:(ch + 1) * 512], in0=PB(b)[:, :], scalar=Wt[:, gt_, ex:ex + 1],
                            in1=acc[:, lt_, ch * 512:(ch + 1) * 512], op0=ALU.mult, op1=ALU.add), [bk(b), "Wt", "acc"], ["acc"])
        for lt_ in range(TGT):
            gt_ = gi * TGT + lt_
            r0 = gt_ * 128
            i = gt_ % 2
            P.dma("sp", x1r[i][:, :], x1s[r0:r0 + 128, :], "x1r%d" % i, reads=["x1s"], writes=["x1r%d" % i])
            P.op("pool", lambda e, i=i, lt_=lt_: e.tensor_tensor(out=x2[i][:, :], in0=acc[:, lt_, :], in1=gt2b[:, :], op=ALU.mult),
                 ["acc", "gt2b"], ["x2%d" % i])
            P.op("dve", lambda e, i=i: e.tensor_tensor(out=x2[i][:, :], in0=x2[i][:, :], in1=x1r[i][:, :], op=ALU.add),
                 ["x2%d" % i, "x1r%d" % i], ["x2%d" % i])
            P.op("act", lambda e, i=i: e.activation(out=junk2[:, :], in_=x2[i][:, :], func=AF.Square, accum_out=stat2[i][:, 0:1]),
                 ["x2%d" % i], ["junk2", "st2%d" % i])
            P.op("dve", lambda e, i=i: e.tensor_scalar(out=stat2[i][:, 1:2], in0=stat2[i][:, 0:1], scalar1=1.0 / D, scalar2=1e-6,
                                                       op0=ALU.mult, op1=ALU.add), ["st2%d" % i], ["st2%d" % i])
            rsq(stat2[i][:, 2:3], stat2[i][:, 1:2], ["st2%d" % i], ["st2%d" % i])
            P.op("dve", lambda e, i=i: e.scalar_tensor_tensor(out=yo[i][:, :], in0=x2[i][:, :], scalar=stat2[i][:, 2:3],
                                                              in1=fgb_t[:, :], op0=ALU.mult, op1=ALU.mult),
                 ["x2%d" % i, "st2%d" % i, "fgb"], ["yo%d" % i])
            P.dma("sp", y[r0:r0 + 128, :], yo[i][:, :], "yst%d" % i, reads=["yo%d" % i], writes=["y"])
    finals = [(k, v) for k, v in P.cnt.items() if k.startswith("D:yst")]
    P.emit(finals)
    stM.close()
    stP.close()
    return nc


def _core_inputs(S, b, j, inp, cst_np):
    import os
    NEH = 1 if os.environ.get("KSTOP") else 32
    f = lambda a: np.ascontiguousarray(a, dtype=np.float32)
    NT = S // 4
    w_in = inp["w_in"][0]
    hq, hf, hi_, hg = 0, 512, 1024, 1536
    rw = 2048
    cj = slice(128 * j, 128 * j + 128)
    rws = lambda o: slice(rw + o + 128 * j, rw + o + 128 * j + 128)
    wfm = np.concatenate([w_in[:, hq + 128 * j:hq + 128 * j + 128], w_in[:, hf + 128 * j:hf + 128 * j + 128],
                          w_in[:, rws(0)], w_in[:, rws(512)], w_in[:, rw + 1536:rw + 1696]], 1)
    wtm = np.concatenate([w_in[:, hi_ + 128 * j:hi_ + 128 * j + 128], w_in[:, hg + 128 * j:hg + 128 * j + 128],
                          w_in[:, rws(1024)]], 1)
    mu = inp["rw_mu"][0]
    mu_fm = np.concatenate([mu[0 + 128 * j:128 * j + 128], mu[512 + 128 * j:512 + 128 * j + 128], mu[1536:1696]])
    mu_tm = mu[1024 + 128 * j:1024 + 128 * j + 128]
    col = lambda v: np.asarray(v, np.float32).reshape(128, 1)
    fmv = lambda v: np.asarray(v, np.float32).reshape(-1, 128).T
    bc = lambda v: np.broadcast_to(np.asarray(v, np.float32)[None, :], (128, len(v)))
    sel = np.zeros(4, np.float32)
    sel[j] = 1
    parts = {
        "c": fmv(inp["c"][b]), "ada_b": fmv(inp["ada_b"][0]), "n1g": fmv(inp["norm1_g"][0]), "n2g": fmv(inp["norm2_g"][0]),
        "lb0": col(inp["hg_lb"][0, cj]), "lb1": col(inp["hg_lb"][1, cj]),
        "w0": col(inp["rw_w0"][0, cj]), "a0": col(inp["rw_a0"][0, cj]), "kk": col(inp["rw_kk"][0, cj]),
        "ka": col(inp["rw_ka"][0, cj]), "rk": col(inp["rw_rk"][0, 2 * j:2 * j + 2, :].reshape(128)),
        "mu_fm": bc(mu_fm), "mu_tm": bc(mu_tm), "hgn": bc(inp["hg_norm_g"][0, cj]),
        "gnw": bc(inp["rw_gn_w"][0, cj]), "gnb": bc(inp["rw_gn_b"][0, cj]),
        "rb": bc(np.concatenate([inp["router_g_b"][0], inp["router_e_b"][0]])), "sel": bc(sel),
    }
    pv = np.concatenate([parts[k] for k, _ in PV_LAYOUT], 1)
    lora = np.zeros((128, 384), np.float32)
    lora[0:32, 0:128] = inp["rw_w2"][0][:, cj]
    lora[0:32, 128:256] = inp["rw_a2"][0][:, cj]
    lora[0:96, 256:384] = inp["rw_g2"][0][:, cj]
    xb = inp["x"][b][:S]
    return {
        "x": f(xb), "cst": cst_np, "pv": f(pv), "ada_w": f(inp["ada_w"][0]), "wfm": f(wfm), "wtm": f(wtm), "lora": lora,
        "wgates": f(w_in[:, 3744:5792]), "wpa": f(inp["w_proj_a"][0]), "wpb": f(inp["w_proj_b"][0]), "wout": f(inp["w_out"][0]),
        "wr": f(np.concatenate([inp["router_g_w"][0], inp["router_e_w"][0]], 1)),
        "eg": f(inp["exp_w_gate"][0][:NEH]), "eu": f(inp["exp_w_up"][0][:NEH]), "ed": f(inp["exp_w_down"][0][:NEH]),
        "fgb": f(bc(inp["final_g"])), "xown": f(xb[j * NT:(j + 1) * NT]),
    }


_NC_CACHE = {}


def run(inp, S, trace=False):
    if S not in _NC_CACHE:
        _NC_CACHE[S] = build(S)
    nc = _NC_CACHE[S]
    cst_np, _ = _consts()
    inp = {k: np.asarray(v) for k, v in inp.items()}
    shared = {}
    in_maps = []
    for core in range(8):
        b, j = core // 4, core % 4
        m = _core_inputs(S, b, j, inp, cst_np)
        for k in ("ada_w", "eg", "eu", "ed", "wgates", "wpa", "wpb", "wout", "wr", "fgb", "cst"):
            m[k] = shared.setdefault(k, m[k])
        in_maps.append(m)
    if trace:
        res = run_bass_kernel_spmd(nc, in_maps, core_ids=list(range(8)), trace=True)
        print("EXEC_NS", res.exec_time_ns)
    else:
        res = run_bass_kernel_spmd(nc, in_maps, core_ids=list(range(8)))
    NT = S // 4
    out = np.zeros((2, S, D), np.float32)
    for core in range(8):
        b, j = core // 4, core % 4
        out[b, j * NT:(j + 1) * NT] = res.results[core]["y"]
    return out


def kernel(**inputs):
    return run(inputs, 8192)
```

```python
import numpy as np
import ml_dtypes
import concourse.bass as bass
import concourse.mybir as mybir
from concourse.bass_utils import run_bass_kernel_spmd

F32 = mybir.dt.float32
BF16 = mybir.dt.bfloat16
ALU = mybir.AluOpType
AF = mybir.ActivationFunctionType
AX = mybir.AxisListType
C0 = float(np.exp(-0.5))
D = 1024


class Prog:
    ENG = ("pe", "act", "dve", "pool", "sp")

    def __init__(self, nc):
        self.nc = nc
        self.ops = {e: [] for e in self.ENG}
        self.cnt = {}
        self.last_w = {}
        self.readers = {}
        self.seen = {e: {} for e in self.ENG}
        self.pending = {e: {} for e in self.ENG}
        self.alias = {}
        self.tot = {}
        self.last_rg = 0
        self.last_pe_tok = None
        self.cap = None
        self.nb = 6
        self.bank_i = 0

    def barrier(self):
        for e in self.ENG:
            self.pending[e] = dict(self.cnt)

    def _deps(self, eng, reads, writes):
        deps = {}

        def add(tok):
            s, v = tok
            if eng == "pe" and s.startswith("E:pe"):
                return
            if self.seen[eng].get(s, 0) >= v:
                return
            if deps.get(s, 0) < v:
                deps[s] = v

        for r in reads:
            if r in self.last_w:
                add(self.last_w[r])
        for w in writes:
            if w in self.last_w:
                add(self.last_w[w])
            for t in self.readers.get(w, ()):
                add(t)
        for s, v in self.pending[eng].items():
            if self.seen[eng].get(s, 0) < v and deps.get(s, 0) < v and not (eng == "pe" and s.startswith("E:pe")):
                deps[s] = v
        self.pending[eng] = {}
        for s, v in deps.items():
            self.seen[eng][s] = v
        return list(deps.items())

    def begin_capture(self, banks):
        self.cap = []
        self.cap_banks = list(banks)
        self.cap_bi = 0

    def end_capture(self):
        c = self.cap
        self.cap = None
        return c

    def replay_merged(self, caps):
        idx = [0] * len(caps)
        total = sum(len(c) for c in caps)
        for _ in range(total):
            best, bf = None, None
            for k, c in enumerate(caps):
                if idx[k] < len(c):
                    f = (idx[k] + 1) / len(c)
                    if bf is None or f < bf:
                        best, bf = k, f
            a = caps[best][idx[best]]
            idx[best] += 1
            self.op(*a)

    def op(self, eng, fn, reads=(), writes=(), sem=None, inc=1, rg=0):
        if self.cap is not None:
            self.cap.append((eng, fn, list(reads), list(writes), sem, inc, rg))
            return None
        reads = [self.alias.get(r, r) for r in reads]
        writes = [self.alias.get(w, w) for w in writes]
        ps_r = [r for r in reads if isinstance(r, tuple) and r[0] == "ps"]
        if ps_r:
            reads = [r for r in reads if not (isinstance(r, tuple) and r[0] == "ps")]
            writes = writes + [r for r in ps_r if r not in writes]
        deps = self._deps(eng, reads, writes)
        if eng == "pe":
            if rg != self.last_rg and self.last_pe_tok is not None:
                ls, lv = self.last_pe_tok
                if not any(d[0] == ls and d[1] >= lv for d in deps):
                    deps = [d for d in deps if d[0] != ls] + [(ls, lv)]
            self.last_rg = rg
        if sem is not None:
            s = sem
        else:
            import os
            ch = int(os.environ.get("KSEMCH", "20000"))
            tot = self.tot.get(eng, 0)
            self.tot[eng] = tot + 1
            s = "E:%s#%d" % (eng, tot // ch)
        self.cnt[s] = self.cnt.get(s, 0) + inc
        tok = (s, self.cnt[s])
        self.ops[eng].append((deps, fn, s, inc))
        if eng == "pe":
            self.last_pe_tok = tok
        for w in writes:
            self.last_w[w] = tok
            self.readers[w] = []
        for r in reads:
            self.readers.setdefault(r, []).append(tok)
        return tok

    def dma(self, eng, out, in_, key, reads=(), writes=()):
        return self.op(eng, lambda e: e.dma_start(out=out, in_=in_), reads, writes, sem="D:" + key, inc=16)

    def bank(self):
        if self.cap is not None:
            b = self.cap_banks[self.cap_bi % len(self.cap_banks)]
            self.cap_bi += 1
            return b
        b = self.bank_i
        self.bank_i = (self.bank_i + 1) % self.nb
        return b

    def emit(self, final_waits):
        nc = self.nc
        names = sorted(self.cnt.keys())
        import contextlib
        with contextlib.ExitStack() as st:
            sems = {n: st.enter_context(nc.semaphore("s%d" % i)) for i, n in enumerate(names)}
            block = st.enter_context(nc.Block())

            def run(eng):
                def f(e):
                    for deps, fn, s, inc in self.ops[eng]:
                        for ds, dv in deps:
                            e.wait_ge(sems[ds], dv)
                        fn(e).then_inc(sems[s], inc)
                    if eng == "sp":
                        for s, v in final_waits:
                            e.wait_ge(sems[s], v)
                return f

            block.tensor(run("pe"))
            block.scalar(run("act"))
            block.vector(run("dve"))
            block.gpsimd(run("pool"))
            block.sync(run("sp"))


def _consts():
    c = {}
    c["ident"] = np.eye(128, dtype=np.float32)
    bo = np.zeros((128, 128), np.float32)
    bo[:64, :64] = 1
    bo[64:, 64:] = 1
    c["bones"] = bo
    c["ones"] = np.ones((128, 128), np.float32)
    hs = np.zeros((128, 2), np.float32)
    hs[:64, 0] = 1
    hs[64:, 1] = 1
    c["hsel"] = hs
    rm = np.ones((128, 512), np.float32)
    rm[:, ::64] = 0
    c["rmask"] = rm
    s = np.arange(64)[:, None]
    t = np.arange(64)[None, :]
    le = (s <= t).astype(np.float32)
    lt = (s < t).astype(np.float32)
    gt = (s > t).astype(np.float32)
    z = np.zeros((64, 1), np.float32)

    def pad(a):
        return np.concatenate([a, np.zeros((128 - a.shape[0], a.shape[1]), np.float32)], 0)

    c["hmask"] = pad(np.tile(le, (1, 8)))
    c["nmask"] = pad(np.tile(np.concatenate([lt, le, lt, le], 1), (1, 2)))
    c["amask"] = pad(np.tile(gt, (1, 4)))
    offs = {}
    o = 0
    arrs = []
    for k, v in c.items():
        offs[k] = (o, v.shape[1])
        o += v.shape[1]
        arrs.append(v)
    return np.ascontiguousarray(np.concatenate(arrs, 1)), offs


PV_LAYOUT = [("c", 8), ("ada_b", 48), ("n1g", 8), ("n2g", 8), ("lb0", 1), ("lb1", 1),
             ("w0", 1), ("a0", 1), ("kk", 1), ("ka", 1), ("rk", 1),
             ("mu_fm", 416), ("mu_tm", 128), ("hgn", 128), ("gnw", 128), ("gnb", 128),
             ("rb", 36), ("sel", 4)]


def _pv_offs():
    o = 0
    d = {}
    for k, n in PV_LAYOUT:
        d[k] = (o, n)
        o += n
    return d, o


def build(S):
    NB = S // 512
    NT = S // 4
    NTT = NT // 128
    TG = min(NT, 1024)
    NG = NT // TG
    cst_np, co = _consts()
    pvo, NV = _pv_offs()
    NCST = cst_np.shape[1]

    nc = bass.Bass("TRN2", target_bir_lowering=False)
    P = Prog(nc)

    def din(name, shape, dt=F32):
        return nc.dram_tensor(name, shape, dt, kind="ExternalInput")

    x = din("x", [S, D])
    cst = din("cst", [128, NCST])
    pv = din("pv", [128, NV])
    ada_w = din("ada_w", [D, 6 * D])
    wfm = din("wfm", [D, 672])
    wtm = din("wtm", [D, 384])
    lora = din("lora", [128, 384])
    wgates = din("wgates", [D, 2048])
    wpa = din("wpa", [512, D])
    wpb = din("wpb", [512, D])
    wout = din("wout", [D, D])
    wr = din("wr", [D, 36])
    import os
    NE = 1 if os.environ.get("KSTOP") else 32
    eg = din("eg", [NE, D, 512])
    eu = din("eu", [NE, D, 512])
    ed = din("ed", [NE, 512, D])
    y = nc.dram_tensor("y", [NT, D], F32, kind="ExternalOutput")
    xin = [nc.dram_tensor("xin%d" % q, [NT, D], BF16) for q in range(4)]
    xout = [nc.dram_tensor("xout%d" % q, [NT, D], BF16) for q in range(4)]
    x1s = nc.dram_tensor("x1s", [NT, D], F32)

    tcount = [0]
    import contextlib
    stP = contextlib.ExitStack()
    cur = [stP]

    def sb(shape, dt=F32, name=None):
        tcount[0] += 1
        return cur[0].enter_context(nc.sbuf_tensor(name or ("t%d" % tcount[0]), list(shape), dt))

    psum = nc.alloc_psum_tensor("psum", [128, 8, 512], F32)

    def PB(b):
        return psum[:, b, :]

    def PBbf(b):
        return psum[:, b, :].bitcast(BF16)

    def bk(b):
        return ("ps", b)

    def rsq(out, in_, rk, wk):
        P.op("act", lambda e: e.activation(out=out, in_=in_, func=AF.Sqrt), rk, wk)
        P.op("dve", lambda e: e.reciprocal(out=out, in_=out), wk, wk)

    CST = sb([128, NCST])
    PV = sb([128, NV])
    P.dma("sp", CST[:, :], cst[:, :], "cst", writes=["CST"])
    P.dma("sp", PV[:, :], pv[:, :], "pv", writes=["PV"])

    def cs(k, rows=128):
        o, n = co[k]
        return CST[0:rows, o:o + n]

    def pvs(k, rows=128):
        o, n = pvo[k]
        return PV[0:rows, o:o + n]

    identb = sb([128, 128], BF16)
    P.op("dve", lambda e: e.tensor_copy(out=identb[:, :], in_=cs("ident")), ["CST"], ["identb"])

    csil = sb([128, 8])
    P.op("act", lambda e: e.activation(out=csil[:, :], in_=pvs("c"), func=AF.Silu), ["PV"], ["csil"])
    mod = sb([128, 48])
    s1 = sb([128, 8])
    s2 = sb([128, 8])
    lbv = sb([128, 4])
    stA = contextlib.ExitStack()
    cur[0] = stA
    W1f = sb([128, 8, 672], BF16)
    W2f = sb([128, 8, 416], BF16)
    W1t = sb([128, 8, 384], BF16)
    W2t = sb([128, 8, 128], BF16)
    lorb = sb([128, 384], BF16)
    stS = contextlib.ExitStack()
    cur[0] = stS
    stg = [sb([128, 8, 512]) for _ in range(2)]
    tmpw = sb([128, 512])
    lor32 = sb([128, 384])
    cur[0] = stA
    adst = stg
    mb = P.bank()
    for g in range(12):
        t = adst[g % 2]
        k = "stg%d" % (g % 2)
        P.dma("sp" if g % 2 == 0 else "act", t[:, :, :],
              ada_w[:, g * 512:(g + 1) * 512].rearrange("(kc p) n -> p kc n", p=128), k, writes=[k])
        for cc in range(4):
            j = g * 4 + cc
            for kc in range(8):
                P.op("pe", lambda e, t=t, cc=cc, kc=kc, j=j: e.matmul(
                    PB(mb)[:, j:j + 1], lhsT=t[:, kc, cc * 128:(cc + 1) * 128], rhs=csil[:, kc:kc + 1],
                    start=(kc == 0), stop=(kc == 7)), [k, "csil"], [bk(mb)])
    P.op("dve", lambda e: e.tensor_tensor(out=mod[:, :], in0=PB(mb)[:, 0:48], in1=pvs("ada_b"), op=ALU.add),
         [bk(mb), "PV"], ["mod"])
    P.op("dve", lambda e: e.scalar_tensor_tensor(out=s1[:, :], in0=mod[:, 8:16], scalar=1.0, in1=pvs("n1g"),
                                                 op0=ALU.add, op1=ALU.mult), ["mod", "PV"], ["s1"])
    P.op("dve", lambda e: e.scalar_tensor_tensor(out=s2[:, :], in0=mod[:, 32:40], scalar=1.0, in1=pvs("n2g"),
                                                 op0=ALU.add, op1=ALU.mult), ["mod", "PV"], ["s2"])
    P.op("dve", lambda e: e.tensor_tensor(out=lbv[:, 0:1], in0=pvs("lb0"), in1=pvs("lb1"), op=ALU.subtract),
         ["PV"], ["lbv"])
    P.op("act", lambda e: e.activation(out=lbv[:, 1:2], in_=lbv[:, 0:1], func=AF.Sigmoid), ["lbv"], ["lbv"])
    P.op("dve", lambda e: e.tensor_scalar(out=lbv[:, 2:3], in0=lbv[:, 1:2], scalar1=-1.0, scalar2=1.0,
                                          op0=ALU.mult, op1=ALU.add), ["lbv"], ["lbv"])
    P.op("dve", lambda e: e.tensor_scalar(out=lbv[:, 3:4], in0=pvs("ka"), scalar1=-1.0, scalar2=1.0,
                                          op0=ALU.mult, op1=ALU.add), ["PV", "lbv"], ["lbv"])
    LB, OML, OMK = lbv[:, 1:2], lbv[:, 2:3], lbv[:, 3:4]

    def stage(i):
        return stg[i % 2], "stg%d" % (i % 2)

    si = [0]

    def load_cast(dst_fn, src_ap, ncols, mu=None, dst2_fn=None, eng_dma="sp"):
        t, k = stage(si[0])
        si[0] += 1
        P.dma(eng_dma, t[:, :, 0:ncols], src_ap.rearrange("(kc p) n -> p kc n", p=128), k, writes=[k])
        for kc in range(8):
            if mu is None:
                P.op("act" if kc % 2 else "dve",
                     (lambda e, kc=kc: e.activation(out=dst_fn(kc), in_=t[:, kc, 0:ncols], func=AF.Copy)) if kc % 2 else
                     (lambda e, kc=kc: e.tensor_copy(out=dst_fn(kc), in_=t[:, kc, 0:ncols])),
                     [k], ["Wa"])
            else:
                P.op("dve", lambda e, kc=kc: e.tensor_tensor(out=tmpw[:, 0:ncols], in0=t[:, kc, 0:ncols], in1=mu,
                                                             op=ALU.mult), [k, "PV"], ["tmpw"])
                P.op("act", lambda e, kc=kc: e.activation(out=dst2_fn(kc), in_=tmpw[:, 0:ncols], func=AF.Copy),
                     ["tmpw"], ["Wa"])
                P.op("dve", lambda e, kc=kc: e.tensor_tensor(out=dst_fn(kc), in0=t[:, kc, 0:ncols],
                                                             in1=tmpw[:, 0:ncols], op=ALU.subtract),
                     [k, "tmpw"], ["Wa"])

    load_cast(lambda kc: W1f[:, kc, 0:256], wfm[:, 0:256], 256)
    load_cast(lambda kc: W1f[:, kc, 256:672], wfm[:, 256:672], 416, mu=pvs("mu_fm"),
              dst2_fn=lambda kc: W2f[:, kc, :], eng_dma="act")
    load_cast(lambda kc: W1t[:, kc, 0:256], wtm[:, 0:256], 256)
    load_cast(lambda kc: W1t[:, kc, 256:384], wtm[:, 256:384], 128, mu=pvs("mu_tm"),
              dst2_fn=lambda kc: W2t[:, kc, :], eng_dma="act")
    P.dma("sp", lor32[:, :], lora[:, :], "lora", writes=["lor32"])
    P.op("dve", lambda e: e.tensor_copy(out=lorb[:, :], in_=lor32[:, :]), ["lor32"], ["Wa"])
    P.barrier()
    stS.close()

    xsl = [sb([128, D]) for _ in range(2)]
    NF = {"junk": sb([128, D], BF16), "xn": [sb([128, D], BF16) for _ in range(2)], "stat": [sb([128, 4]) for _ in range(2)]}
    nti = [0]

    def load_x_tile(row0):
        i = nti[0] % 2
        P.dma("sp", xsl[i][:, :], x[row0:row0 + 128, :], "x%d" % i, writes=["xs%d" % i])
        return xsl[i], "xs%d" % i

    def norm_fm(xt, xk, sc, bi, sck, out_fn, outk):
        i = nti[0] % 2
        nti[0] += 1
        junk, xn, stat = NF["junk"], NF["xn"], NF["stat"]
        st_, sk = stat[i], "stat%d" % i
        P.op("act", lambda e: e.activation(out=junk[:, :], in_=xt[:, :], func=AF.Square, accum_out=st_[:, 0:1]),
             [xk], ["junk", sk])
        P.op("dve", lambda e: e.tensor_scalar(out=st_[:, 1:2], in0=st_[:, 0:1], scalar1=1.0 / D, scalar2=1e-6,
                                              op0=ALU.mult, op1=ALU.add), [sk], [sk])
        rsq(st_[:, 2:3], st_[:, 1:2], [sk], [sk])
        xnt, xnk = xn[i], "xn%d" % i
        P.op("dve", lambda e: e.tensor_scalar(out=xnt[:, :], in0=xt[:, :], scalar1=st_[:, 2:3], scalar2=None,
                                              op0=ALU.mult), [xk, sk], [xnk])
        b = P.bank()
        for kc in range(8):
            P.op("pe", lambda e, kc=kc: e.transpose(out=PBbf(b)[:, kc * 128:(kc + 1) * 128],
                                                    in_=xnt[:, kc * 128:(kc + 1) * 128], identity=identb[:, :]),
                 [xnk, "identb"], [bk(b)])
        for kc in range(8):
            if kc % 2:
                P.op("act", lambda e, kc=kc: e.activation(out=out_fn(kc), in_=PBbf(b)[:, kc * 128:(kc + 1) * 128],
                                                          func=AF.Identity, scale=sc[:, kc:kc + 1],
                                                          bias=bi[:, kc:kc + 1]), [bk(b), sck], [outk])
            else:
                P.op("dve", lambda e, kc=kc: e.tensor_scalar(out=out_fn(kc), in0=PBbf(b)[:, kc * 128:(kc + 1) * 128],
                                                             scalar1=sc[:, kc:kc + 1], scalar2=bi[:, kc:kc + 1],
                                                             op0=ALU.mult, op1=ALU.add), [bk(b), sck], [outk])
        return st_, sk

    hTs = [sb([128, 8, 512], BF16)] * 2
    hSh = [sb([128, 8, 512], BF16)] * 2
    BIG = lambda dt=F32: sb([128, 512], dt)
    h_f, h_lf, h_kk, h_q, h_cum, h_e, h_t = BIG(), BIG(), BIG(), BIG(), BIG(), BIG(), BIG()
    hq2, hf2 = BIG(), BIG()
    h_qhat, h_qtl, h_ktl, h_khat = BIG(BF16), BIG(BF16), BIG(BF16), BIG(BF16)
    h_khT = sb([64, 8, 128], BF16)
    h_scT = sb([64, 8, 64], BF16)
    Vh = sb([64, 8, 128], BF16)
    SGhL = [sb([64, 8, 128], BF16) for _ in range(2)]
    VrL = [sb([64, 8, 128], BF16) for _ in range(2)]
    S32 = sb([128, 128])
    Sbf = [sb([128, 128], BF16) for _ in range(2)]
    h_o = sb([64, 8, 128])
    h_sq = sb([64, 8, 128])
    h_st = sb([64, 32])
    oa = sb([64, 8, 128], BF16)
    r_tw, r_ad, r_sg = sb([32, 512], BF16), sb([32, 512], BF16), sb([96, 512], BF16)
    r_sw, r_a, r_cw, r_cx, r_dl, r_Ea, r_El = h_f, h_lf, h_kk, h_q, h_cum, h_e, h_t
    for a_, b_ in (("r_sw", "h_f"), ("r_a", "h_lf"), ("r_cw", "h_kk"), ("r_cx", "h_q"), ("r_dl", "h_cum"),
                   ("r_Ea", "h_e"), ("r_El", "h_t"), ("r_t1", "r_sq"), ("r_bv", "r_kkv")):
        P.alias[a_] = b_
    r_Er, r_En = BIG(), BIG()
    r_k, r_r, r_kkv, r_sq, r_kkn, r_k2, r_rt32 = (BIG() for _ in range(7))
    r_prod = r_k
    P.alias["r_prod"] = "r_k"
    r_t1, r_bv = r_sq, r_kkv
    r_bh, r_kh = BIG(BF16), BIG(BF16)
    FM = sb([128, 8, 4, 64], BF16)
    TM = sb([64, 8, 3, 128], BF16)
    bonL = [sb([64, 8, 2]) for _ in range(2)]
    GtL = [sb([64, 8, 128], BF16) for _ in range(2)]
    NmL = [sb([64, 4, 4, 64], BF16) for _ in range(4)]
    AkpL = [[sb([64, 4, 64], BF16) for _ in range(2)] for _ in range(4)]
    NkpL = [[sb([64, 4, 64], BF16) for _ in range(2)] for _ in range(4)]
    Z32L = [sb([64, 4, 128]) for _ in range(4)]
    ZbfL = [sb([64, 4, 128], BF16) for _ in range(4)]
    RhTL = [sb([128, 2, 64], BF16) for _ in range(4)]
    MTL = [sb([128, 2, 64], BF16) for _ in range(4)]
    H32 = sb([128, 64])
    Hbf = [sb([128, 64], BF16) for _ in range(2)]
    Yt = sb([64, 8, 128])
    r_ysq = h_sq
    P.alias["r_ysq"] = "h_sq"
    r_st = sb([64, 80])
    r_tmp = h_o
    P.alias["r_tmp"] = "h_o"
    ob = sb([64, 8, 128], BF16)
    osel = sb([64, 8, 4, 256], BF16)

    P.op("pool", lambda e: e.memset(S32[:, :], 0.0), [], ["S32"])
    P.op("pool", lambda e: e.memset(Sbf[0][:, :], 0.0), [], ["Sbf0"])
    P.op("pool", lambda e: e.memset(H32[:, :], 0.0), [], ["H32"])
    P.op("pool", lambda e: e.memset(Hbf[0][:, :], 0.0), [], ["Hbf0"])
    def v3(t, n=64):
        return t[:, :].rearrange("p (c t) -> p c t", t=n)

    state = {"sidx": 0, "hidx": 0}

    import os
    KSTOP = os.environ.get("KSTOP", "")

    class _Stop(Exception):
        pass

    def chk(tag):
        if KSTOP == tag:
            raise _Stop()

    RDG = {}
    EXTRA = {"cap": None}

    def phaseA_out(blk):
        ob_banks = [6, 7]
        par = blk % 2
        SGh, Vr, Gt, bon = SGhL[par], VrL[par], GtL[par], bonL[par]
        kSGh, kVr, kGt, kbon = "SGh%d" % par, "Vr%d" % par, "Gt%d" % par, "bon%d" % par
        chk("A5")
        for half in range(2):
            P.op("act", lambda e, half=half: e.activation(
                out=h_o[:, half * 4:half * 4 + 4, :].rearrange("p c k -> p (c k)"), in_=PB(ob_banks[half])[0:64, :],
                func=AF.Copy), [bk(ob_banks[half])], ["h_o"])
        P.op("pool", lambda e: e.tensor_tensor(out=h_sq[:, :, :], in0=h_o[:, :, :], in1=h_o[:, :, :], op=ALU.mult),
             ["h_o"], ["h_sq"])
        P.op("dve", lambda e: e.tensor_reduce(out=h_st[:, 0:8], in_=h_sq[:, :, :], axis=AX.X, op=ALU.add), ["h_sq"], ["h_st"])
        P.op("dve", lambda e: e.tensor_scalar(out=h_st[:, 8:16], in0=h_st[:, 0:8], scalar1=1.0 / 128, scalar2=1e-6,
                                              op0=ALU.mult, op1=ALU.add), ["h_st"], ["h_st"])
        rsq(h_st[:, 16:24], h_st[:, 8:16], ["h_st"], ["h_st"])
        P.op("dve", lambda e: e.tensor_tensor(out=h_o[:, :, :], in0=h_o[:, :, :],
                                              in1=h_st[:, 16:24].unsqueeze(2).to_broadcast([64, 8, 128]), op=ALU.mult),
             ["h_o", "h_st"], ["h_o"])
        P.op("pool", lambda e: e.tensor_tensor(out=h_sq[:, :, :], in0=SGh[:, :, :],
                                               in1=pvs("hgn", 64).unsqueeze(1).to_broadcast([64, 8, 128]), op=ALU.mult),
             [kSGh, "PV", "h_st"], ["h_sq"])
        P.op("dve", lambda e: e.tensor_tensor(out=oa[:, :, :], in0=h_o[:, :, :], in1=h_sq[:, :, :], op=ALU.mult),
             ["h_o", "h_sq"], ["oa"])
        q = (blk * 512) // NT
        r0 = blk * 512 - q * NT
        selb = pvs("sel", 64).unsqueeze(1).unsqueeze(3).to_broadcast([64, 8, 4, 128])
        P.op("dve", lambda e: e.tensor_tensor(out=osel[:, :, :, 0:128], in0=oa[:, :, :].unsqueeze(2).to_broadcast([64, 8, 4, 128]),
                                              in1=selb, op=ALU.mult), ["oa", "PV"], ["osel"])

        chk("A6")
        Y4 = Yt[:, :, :].rearrange("p c (h k) -> p (c h) k", k=64)
        P.op("dve", lambda e: e.tensor_reduce(out=r_st[:, 0:16], in_=Y4, axis=AX.X, op=ALU.add), ["Yt"], ["r_st"])
        P.op("pool", lambda e: e.tensor_tensor(out=r_ysq[:, :, :], in0=Yt[:, :, :], in1=Yt[:, :, :], op=ALU.mult),
             ["Yt"], ["r_ysq"])
        P.op("dve", lambda e: e.tensor_reduce(out=r_st[:, 16:32], in_=r_ysq[:, :, :].rearrange("p c (h k) -> p (c h) k", k=64),
                                              axis=AX.X, op=ALU.add), ["r_ysq"], ["r_st"])
        P.op("dve", lambda e: e.tensor_scalar(out=r_st[:, 0:16], in0=r_st[:, 0:16], scalar1=1.0 / 64, scalar2=None,
                                              op0=ALU.mult), ["r_st"], ["r_st"])
        P.op("dve", lambda e: e.tensor_tensor(out=r_st[:, 32:48], in0=r_st[:, 0:16], in1=r_st[:, 0:16], op=ALU.mult),
             ["r_st"], ["r_st"])
        P.op("dve", lambda e: e.scalar_tensor_tensor(out=r_st[:, 48:64], in0=r_st[:, 16:32], scalar=1.0 / 64,
                                                     in1=r_st[:, 32:48], op0=ALU.mult, op1=ALU.subtract),
             ["r_st"], ["r_st"])
        P.op("dve", lambda e: e.tensor_scalar(out=r_st[:, 64:80], in0=r_st[:, 48:64], scalar1=64e-5, scalar2=None,
                                              op0=ALU.add), ["r_st"], ["r_st"])
        rsq(r_st[:, 64:80], r_st[:, 64:80], ["r_st"], ["r_st"])
        T4 = r_tmp[:, :, :].rearrange("p c (h k) -> p (c h) k", k=64)
        P.op("dve", lambda e: e.tensor_tensor(out=T4, in0=Y4, in1=r_st[:, 0:16].unsqueeze(2).to_broadcast([64, 16, 64]),
                                              op=ALU.subtract), ["Yt", "r_st"], ["r_tmp"])
        P.op("dve", lambda e: e.tensor_tensor(out=T4, in0=T4, in1=r_st[:, 64:80].unsqueeze(2).to_broadcast([64, 16, 64]),
                                              op=ALU.mult), ["r_tmp", "r_st"], ["r_tmp"])
        P.op("pool", lambda e: e.tensor_tensor(out=r_tmp[:, :, :], in0=r_tmp[:, :, :],
                                               in1=pvs("gnw", 64).unsqueeze(1).to_broadcast([64, 8, 128]), op=ALU.mult),
             ["r_tmp", "PV"], ["r_tmp"])
        P.op("pool", lambda e: e.tensor_tensor(out=r_tmp[:, :, :], in0=r_tmp[:, :, :],
                                               in1=pvs("gnb", 64).unsqueeze(1).to_broadcast([64, 8, 128]), op=ALU.add),
             ["r_tmp", "PV"], ["r_tmp"])
        Q4 = r_ysq[:, :, :].rearrange("p c (h k) -> p (c h) k", k=64)
        P.op("dve", lambda e: e.tensor_tensor(out=Q4, in0=Vr[:, :, :].rearrange("p c (h k) -> p (c h) k", k=64),
                                              in1=bon[:, :, :].rearrange("p c h -> p (c h)").unsqueeze(2).to_broadcast([64, 16, 64]),
                                              op=ALU.mult), [kVr, kbon, "r_st"], ["r_ysq"])
        P.op("dve", lambda e: e.tensor_tensor(out=r_tmp[:, :, :], in0=r_tmp[:, :, :], in1=r_ysq[:, :, :], op=ALU.add),
             ["r_tmp", "r_ysq"], ["r_tmp"])
        P.op("dve", lambda e: e.tensor_tensor(out=ob[:, :, :], in0=r_tmp[:, :, :], in1=Gt[:, :, :], op=ALU.mult),
             ["r_tmp", kGt], ["ob"])
        P.op("dve", lambda e: e.tensor_tensor(out=osel[:, :, :, 128:256], in0=ob[:, :, :].unsqueeze(2).to_broadcast([64, 8, 4, 128]),
                                              in1=selb, op=ALU.mult), ["ob", "PV"], ["osel"])
        PS = min(512, NT)
        for p_ in range(512 // PS):
            tok0 = blk * 512 + p_ * PS
            q = tok0 // NT
            r0 = tok0 - q * NT
            cA, cB = p_ * PS // 64, (p_ + 1) * PS // 64
            P.dma("sp", xin[q][r0:r0 + PS, :].rearrange("(c t) v -> t c v", t=64),
                  osel[:, cA:cB, :, :].rearrange("p c s v -> p c (s v)"), "ost", reads=["osel"], writes=["xin%d" % q])
        return q

    def phaseA_block(blk, part):
        ob_banks = [6, 7]
        par = blk % 2
        SGh, Vr, Gt, bon = SGhL[par], VrL[par], GtL[par], bonL[par]
        kSGh, kVr, kGt, kbon = "SGh%d" % par, "Vr%d" % par, "Gt%d" % par, "bon%d" % par
        hi = 0
        hT, hk = hTs[hi], "hT%d" % hi
        hS, shk = hSh[hi], "hS%d" % hi
        if part == "norm":
            if blk == 0:
                P.op("pool", lambda e: e.memset(hS[:, :, 0:1], 0.0), [], [shk])
            else:
                P.op("pool", lambda e: e.tensor_copy(out=hS[:, :, 0:1], in_=hT[:, :, 511:512]), [hk], [shk])
            for tt in range(4):
                xt, xk = load_x_tile(blk * 512 + tt * 128)
                norm_fm(xt, xk, s1, mod, "s1", lambda kc, tt=tt: hT[:, kc, tt * 128:(tt + 1) * 128], hk)
            P.op("act", lambda e: e.activation(out=hS[:, 0:4, 1:512], in_=hT[:, 0:4, 0:511], func=AF.Copy), [hk, shk], [shk])
            P.op("dve", lambda e: e.tensor_copy(out=hS[:, 4:8, 1:512], in_=hT[:, 4:8, 0:511]), [hk, shk], [shk])
            return

        def proj_fm(off, n, shifted):
            b = P.bank()
            nmm = 16 if shifted else 8
            i = 0
            for kc in range(8):
                P.op("pe", lambda e, kc=kc, i=i: e.matmul(PB(b)[0:n, :], lhsT=W1f[:, kc, off:off + n],
                                                          rhs=hT[:, kc, :], start=(i == 0), stop=(i == nmm - 1)),
                     [hk, "Wa"], [bk(b)])
                i += 1
                if shifted:
                    P.op("pe", lambda e, kc=kc, i=i: e.matmul(PB(b)[0:n, :], lhsT=W2f[:, kc, off - 256:off - 256 + n],
                                                              rhs=hS[:, kc, :], start=False, stop=(i == nmm - 1)),
                         [shk, "Wa"], [bk(b)])
                    i += 1
            return b

        if part == "front2":
            chk("A1")
            KSUB = int(os.environ.get("KSUB", "9"))
            for c in range(8):
                b = P.bank()
                for kc in range(8):
                    P.op("pe", lambda e, kc=kc, c=c, b=b: e.matmul(PB(b)[0:64, 0:384], lhsT=hT[:, kc, c * 64:64 + c * 64],
                                                                   rhs=W1t[:, kc, :], start=(kc == 0), stop=False),
                         [hk, "Wa"], [bk(b)])
                if KSUB >= 2:
                    for kc in range(8):
                        P.op("pe", lambda e, kc=kc, c=c, b=b: e.matmul(PB(b)[0:64, 256:384], lhsT=hS[:, kc, c * 64:64 + c * 64],
                                                                       rhs=W2t[:, kc, :], start=False, stop=(kc == 7)),
                             [shk, "Wa"], [bk(b)])
                if KSUB >= 3:
                    P.op("dve", lambda e, c=c, b=b: e.tensor_copy(out=Vh[:, c, :], in_=PB(b)[0:64, 0:128]), [bk(b)], ["Vh"])
                if KSUB >= 4:
                    P.op("act", lambda e, c=c, b=b: e.activation(out=SGh[:, c, :], in_=PB(b)[0:64, 128:256], func=AF.Silu),
                         [bk(b)], [kSGh])
                if KSUB >= 5:
                    P.op("dve", lambda e, c=c, b=b: e.tensor_copy(out=Vr[:, c, :], in_=PB(b)[0:64, 256:384]), [bk(b)], [kVr])

            chk("A3")
            br = proj_fm(256, 128, True)
            P.op("act", lambda e: e.activation(out=r_r[:, :], in_=PB(br)[:, :], func=AF.Copy), [bk(br)], ["r_r"])
            bkk = proj_fm(384, 128, True)
            P.op("act", lambda e: e.activation(out=r_k[:, :], in_=PB(bkk)[:, :], func=AF.Copy), [bk(bkk)], ["r_k"])
            P.op("dve", lambda e: e.tensor_scalar(out=r_kkv[:, :], in0=PB(bkk)[:, :], scalar1=pvs("kk"), scalar2=None,
                                                  op0=ALU.mult), [bk(bkk), "PV"], ["r_kkv"])
            bwd = proj_fm(512, 32, True)
            P.op("act", lambda e: e.activation(out=r_tw[:, :], in_=PB(bwd)[0:32, :], func=AF.Tanh), [bk(bwd)], ["r_tw"])
            bad = proj_fm(544, 32, True)
            P.op("dve", lambda e: e.tensor_copy(out=r_ad[:, :], in_=PB(bad)[0:32, :]), [bk(bad)], ["r_ad"])
            bgd = proj_fm(576, 96, True)
            P.op("act", lambda e: e.activation(out=r_sg[:, :], in_=PB(bgd)[0:96, :], func=AF.Sigmoid), [bk(bgd)], ["r_sg"])
            bW = P.bank()
            P.op("pe", lambda e: e.matmul(PB(bW)[:, :], lhsT=lorb[0:32, 0:128], rhs=r_tw[:, :], start=True, stop=True),
                 ["Wa", "r_tw"], [bk(bW)])
            P.op("act", lambda e: e.activation(out=r_sw[:, :], in_=PB(bW)[:, :], func=AF.Sigmoid, bias=pvs("w0")),
                 [bk(bW), "PV"], ["r_sw"])
            bA = P.bank()
            P.op("pe", lambda e: e.matmul(PB(bA)[:, :], lhsT=lorb[0:32, 128:256], rhs=r_ad[:, :], start=True, stop=True),
                 ["Wa", "r_ad"], [bk(bA)])
            P.op("act", lambda e: e.activation(out=r_a[:, :], in_=PB(bA)[:, :], func=AF.Sigmoid, bias=pvs("a0")),
                 [bk(bA), "PV"], ["r_a"])
            P.op("pool", lambda e: e.tensor_tensor(out=r_sq[:, :], in0=r_kkv[:, :], in1=r_kkv[:, :], op=ALU.mult),
                 ["r_kkv"], ["r_sq"])
            bN = P.bank()
            P.op("pe", lambda e: e.matmul(PB(bN)[:, :], lhsT=cs("bones"), rhs=r_sq[:, :], start=True, stop=True),
                 ["CST", "r_sq"], [bk(bN)])
            P.op("dve", lambda e: e.tensor_scalar(out=r_sq[:, :], in0=PB(bN)[:, :], scalar1=1e-24, scalar2=None,
                                                  op0=ALU.max), [bk(bN)], ["r_sq"])
            rsq(r_sq[:, :], r_sq[:, :], ["r_sq"], ["r_sq"])
            P.op("dve", lambda e: e.tensor_tensor(out=r_kkn[:, :], in0=r_kkv[:, :], in1=r_sq[:, :], op=ALU.mult),
                 ["r_kkv", "r_sq"], ["r_kkn"])
            P.op("dve", lambda e: e.tensor_scalar(out=r_t1[:, :], in0=r_a[:, :], scalar1=pvs("ka"), scalar2=OMK,
                                                  op0=ALU.mult, op1=ALU.add), ["r_a", "PV", "lbv"], ["r_t1"])
            P.op("pool", lambda e: e.tensor_tensor(out=r_k2[:, :], in0=r_k[:, :], in1=r_t1[:, :], op=ALU.mult),
                 ["r_k", "r_t1"], ["r_k2"])
            P.op("pool", lambda e: e.tensor_tensor(out=r_bv[:, :], in0=r_kkn[:, :], in1=r_a[:, :], op=ALU.mult),
                 ["r_kkn", "r_a"], ["r_bv"])
            P.op("dve", lambda e: e.tensor_tensor_scan(out=r_cw[:, :], data0=cs("rmask"), data1=r_sw[:, :], initial=0.0,
                                                       op0=ALU.mult, op1=ALU.add), ["r_sw", "CST"], ["r_cw"])
            P.op("pool", lambda e: e.tensor_tensor(out=r_cx[:, :], in0=r_cw[:, :], in1=r_sw[:, :], op=ALU.subtract),
                 ["r_cw", "r_sw"], ["r_cx"])
            P.op("dve", lambda e: e.tensor_tensor(out=v3(r_dl), in0=v3(r_cw)[:, :, 63:64].to_broadcast([128, 8, 64]),
                                                  in1=v3(r_cw), op=ALU.subtract), ["r_cw"], ["r_dl"])
            P.op("act", lambda e: e.activation(out=r_Er[:, :], in_=r_cw[:, :], func=AF.Exp, scale=-C0), ["r_cw"], ["r_Er"])
            P.op("act", lambda e: e.activation(out=r_En[:, :], in_=r_cw[:, :], func=AF.Exp, scale=C0), ["r_cw"], ["r_En"])
            P.op("act", lambda e: e.activation(out=r_Ea[:, :], in_=r_cx[:, :], func=AF.Exp, scale=-C0), ["r_cx"], ["r_Ea"])
            P.op("act", lambda e: e.activation(out=r_El[:, :], in_=r_dl[:, :], func=AF.Exp, scale=-C0), ["r_dl"], ["r_El"])
            r_dg = sb([128, 8], name="r_dg%d" % blk)
            RDG[blk] = r_dg
            P.op("pool", lambda e: e.tensor_copy(out=r_dg[:, :], in_=v3(r_Er)[:, :, 63]), ["r_Er"], ["r_dg"])
            P.op("dve", lambda e: e.scalar_tensor_tensor(out=FM[:, :, 0, :], in0=v3(r_kkn), scalar=-1.0, in1=v3(r_Ea),
                                                         op0=ALU.mult, op1=ALU.mult), ["r_kkn", "r_Ea"], ["FM"])
            P.op("dve", lambda e: e.tensor_tensor(out=r_rt32[:, :], in0=r_r[:, :], in1=r_Er[:, :], op=ALU.mult),
                 ["r_r", "r_Er"], ["r_rt32"])
            P.op("act", lambda e: e.activation(out=FM[:, :, 1, :], in_=v3(r_rt32), func=AF.Copy), ["r_rt32"], ["FM"])
            P.op("dve", lambda e: e.tensor_tensor(out=FM[:, :, 2, :], in0=v3(r_bv), in1=v3(r_En), op=ALU.mult),
                 ["r_bv", "r_En"], ["FM"])
            P.op("pool", lambda e: e.tensor_tensor(out=FM[:, :, 3, :], in0=v3(r_k2), in1=v3(r_En), op=ALU.mult),
                 ["r_k2", "r_En"], ["FM"])
            P.op("dve", lambda e: e.tensor_tensor(out=r_bh[:, :], in0=r_bv[:, :], in1=r_El[:, :], op=ALU.mult),
                 ["r_bv", "r_El"], ["r_bh"])
            P.op("pool", lambda e: e.tensor_tensor(out=r_kh[:, :], in0=r_k2[:, :], in1=r_El[:, :], op=ALU.mult),
                 ["r_k2", "r_El"], ["r_kh"])
            P.op("dve", lambda e: e.scalar_tensor_tensor(out=r_prod[:, :], in0=r_r[:, :], scalar=pvs("rk"), in1=r_k2[:, :],
                                                         op0=ALU.mult, op1=ALU.mult), ["r_r", "r_k2", "PV"], ["r_prod"])
            for cp in range(4):
                b = P.bank()
                for cc in range(2):
                    c = cp * 2 + cc
                    for i, (src, sk) in enumerate(((None, "FM"), (r_bh, "r_bh"), (r_kh, "r_kh"))):
                        in_ap = FM[:, c, 0, :] if src is None else src[:, c * 64:(c + 1) * 64]
                        P.op("pe", lambda e, in_ap=in_ap, cc=cc, i=i, b=b: e.transpose(
                            out=PBbf(b)[0:64, (cc * 3 + i) * 128:(cc * 3 + i + 1) * 128], in_=in_ap, identity=identb[:, :]),
                             [sk, "identb"], [bk(b)])
                P.op("act", lambda e, cp=cp, b=b: e.activation(
                    out=TM[:, cp * 2:cp * 2 + 2, :, :].rearrange("p c i k -> p (c i k)"), in_=PBbf(b)[0:64, 0:768],
                    func=AF.Copy), [bk(b)], ["TM"])
            bb = P.bank()
            for c in range(8):
                P.op("pe", lambda e, c=c: e.matmul(PB(bb)[0:64, c * 2:c * 2 + 2], lhsT=r_prod[:, c * 64:(c + 1) * 64],
                                                   rhs=cs("hsel"), start=True, stop=True), ["r_prod", "CST"], [bk(bb)])
            P.op("dve", lambda e: e.tensor_copy(out=bon[:, :, :].rearrange("p c h -> p (c h)"), in_=PB(bb)[0:64, 0:16]),
                 [bk(bb)], [kbon])
            for half in range(2):
                b = P.bank()
                for cc in range(4):
                    c = half * 4 + cc
                    P.op("pe", lambda e, c=c, cc=cc, b=b: e.matmul(PB(b)[0:64, cc * 128:(cc + 1) * 128],
                                                                   lhsT=r_sg[:, c * 64:(c + 1) * 64], rhs=lorb[0:96, 256:384],
                                                                   start=True, stop=True), ["r_sg", "Wa"], [bk(b)])
                P.op("act", lambda e, half=half, b=b: e.activation(
                    out=Gt[:, half * 4:half * 4 + 4, :].rearrange("p c k -> p (c k)"), in_=PB(b)[0:64, :], func=AF.Copy),
                     [bk(b)], [kGt])

            bq = proj_fm(0, 128, False)
            P.op("act", lambda e: e.activation(out=hq2[:, :], in_=PB(bq)[:, :], func=AF.Silu), [bk(bq)], ["hq2"])
            bf_ = proj_fm(128, 128, False)
            P.op("act", lambda e: e.activation(out=hf2[:, :], in_=PB(bf_)[:, :], func=AF.Sigmoid), [bk(bf_)], ["hf2"])
            return None
        r_dg = RDG[blk]
        P.begin_capture([4])
        chk("A2")
        P.op("dve", lambda e: e.tensor_scalar(out=h_f[:, :], in0=hf2[:, :], scalar1=OML, scalar2=LB,
                                              op0=ALU.mult, op1=ALU.add), ["hf2", "lbv"], ["h_f"])
        chk("H2")
        P.op("act", lambda e: e.activation(out=h_lf[:, :], in_=h_f[:, :], func=AF.Ln), ["h_f"], ["h_lf"])
        P.op("pool", lambda e: e.tensor_scalar(out=h_kk[:, :], in0=h_f[:, :], scalar1=-1.0, scalar2=1.0,
                                               op0=ALU.mult, op1=ALU.add), ["h_f"], ["h_kk"])
        chk("H3")
        P.op("dve", lambda e: e.tensor_tensor_scan(out=h_cum[:, :], data0=cs("rmask"), data1=h_lf[:, :], initial=0.0,
                                                   op0=ALU.mult, op1=ALU.add), ["h_lf", "CST"], ["h_cum"])
        chk("H4")
        P.op("act", lambda e: e.activation(out=h_e[:, :], in_=h_cum[:, :], func=AF.Exp), ["h_cum"], ["h_e"])
        P.op("dve", lambda e: e.tensor_tensor(out=h_qhat[:, :], in0=hq2[:, :], in1=h_e[:, :], op=ALU.mult),
             ["hq2", "h_e"], ["h_qhat"])
        chk("H5")
        h_dg = sb([128, 8], name="h_dg%d" % blk)
        P.op("pool", lambda e: e.tensor_copy(out=h_dg[:, :], in_=v3(h_e)[:, :, 63]), ["h_e"], ["h_dg"])
        chk("H6")
        P.op("dve", lambda e: e.tensor_tensor(out=v3(h_t), in0=v3(h_cum), in1=v3(h_cum)[:, :, 31:32].to_broadcast([128, 8, 64]),
                                              op=ALU.subtract), ["h_cum"], ["h_t"])
        P.op("act", lambda e: e.activation(out=h_e[:, :], in_=h_t[:, :], func=AF.Exp), ["h_t", "h_dg"], ["h_e"])
        P.op("dve", lambda e: e.tensor_tensor(out=h_qtl[:, :], in0=hq2[:, :], in1=h_e[:, :], op=ALU.mult),
             ["hq2", "h_e"], ["h_qtl"])
        P.op("act", lambda e: e.activation(out=h_e[:, :], in_=h_t[:, :], func=AF.Exp, scale=-1.0), ["h_t", "h_qtl"], ["h_e"])
        P.op("dve", lambda e: e.tensor_tensor(out=h_ktl[:, :], in0=h_kk[:, :], in1=h_e[:, :], op=ALU.mult),
             ["h_kk", "h_e"], ["h_ktl"])
        chk("H7")
        P.op("dve", lambda e: e.tensor_tensor(out=v3(h_t), in0=v3(h_cum)[:, :, 63:64].to_broadcast([128, 8, 64]),
                                              in1=v3(h_cum), op=ALU.subtract), ["h_cum", "h_e"], ["h_t"])
        P.op("act", lambda e: e.activation(out=h_e[:, :], in_=h_t[:, :], func=AF.Exp), ["h_t", "h_ktl"], ["h_e"])
        P.op("dve", lambda e: e.tensor_tensor(out=h_khat[:, :], in0=h_kk[:, :], in1=h_e[:, :], op=ALU.mult),
             ["h_kk", "h_e"], ["h_khat"])
        chk("H8")
        bt_ = P.bank()
        for c in range(8):
            P.op("pe", lambda e, c=c: e.transpose(out=PBbf(bt_)[0:64, c * 128:(c + 1) * 128],
                                                  in_=h_khat[:, c * 64:(c + 1) * 64], identity=identb[:, :]),
                 ["h_khat", "identb"], [bk(bt_)])
        P.op("act", lambda e: e.activation(out=h_khT[:, :, :].rearrange("p c k -> p (c k)"), in_=PBbf(bt_)[0:64, :],
                                           func=AF.Copy), [bk(bt_)], ["h_khT"])
        chk("H9")
        bs_ = P.bank()
        for c in range(8):
            P.op("pe", lambda e, c=c: e.matmul(PB(bs_)[0:64, c * 64:(c + 1) * 64], lhsT=h_ktl[:, c * 64:(c + 1) * 64],
                                               rhs=h_qtl[:, c * 64:(c + 1) * 64], start=True, stop=True),
                 ["h_ktl", "h_qtl"], [bk(bs_)])
        chk("H10")
        P.op("dve", lambda e: e.tensor_tensor(out=h_scT[:, :, :].rearrange("p c t -> p (c t)"), in0=cs("hmask", 64),
                                              in1=PB(bs_)[0:64, :], op=ALU.mult), [bk(bs_), "CST"], ["h_scT"])
        capH = P.end_capture()
        P.begin_capture([0, 1, 2, 3])
        chk("A4")
        ob_banks = [6, 7]
        for pg in range(4):
            c0 = pg * 2
            Nm, nmk = NmL[pg], "Nm%d" % pg
            Z32, z32k, Zbf, zbk = Z32L[pg], "Z32%d" % pg, ZbfL[pg], "Zbf%d" % pg
            Akp, Nkp = AkpL[pg], NkpL[pg]
            for cc in range(2):
                c = c0 + cc
                b = P.bank()
                for h in range(2):
                    ph = slice(h * 64, (h + 1) * 64)
                    rhs = FM[ph, c, 0:2, :].rearrange("p a t -> p (a t)")
                    P.op("pe", lambda e, ph=ph, c=c, h=h, b=b, rhs=rhs: e.matmul(
                        PB(b)[0:64, h * 256:h * 256 + 128], lhsT=FM[ph, c, 2, :], rhs=rhs, start=True, stop=True),
                         ["FM"], [bk(b)], rg=h * 64)
                    P.op("pe", lambda e, ph=ph, c=c, h=h, b=b, rhs=rhs: e.matmul(
                        PB(b)[0:64, h * 256 + 128:h * 256 + 256], lhsT=FM[ph, c, 3, :], rhs=rhs, start=True, stop=True),
                         ["FM"], [bk(b)], rg=h * 64)
                P.op("dve", lambda e, cc=cc, b=b, Nm=Nm: e.tensor_tensor(
                    out=Nm[:, cc * 2:cc * 2 + 2, :, :].rearrange("p u i t -> p (u i t)"), in0=cs("nmask", 64),
                    in1=PB(b)[0:64, :], op=ALU.mult), [bk(b), "CST"], [nmk])
            b = P.bank()
            for h in range(2):
                for cc in range(2):
                    u = cc * 2 + h
                    c = c0 + cc
                    ph = slice(h * 64, (h + 1) * 64)
                    P.op("pe", lambda e, u=u, c=c, ph=ph, b=b: e.matmul(PB(b)[0:64, u * 64:(u + 1) * 64], lhsT=FM[ph, c, 0, :],
                                                                        rhs=FM[ph, c, 2, :], start=True, stop=True),
                         ["FM"], [bk(b)], rg=h * 64)
            for u in range(4):
                cc, h = u // 2, u % 2
                c = c0 + cc
                P.op("pe", lambda e, u=u, c=c, h=h, b=b, Nm=Nm: e.matmul(PB(b)[0:64, 256 + u * 64:256 + (u + 1) * 64],
                                                                         lhsT=Nm[:, u, 2, :], rhs=Vr[:, c, h * 64:(h + 1) * 64],
                                                                         start=True, stop=True), [nmk, kVr], [bk(b)])
            P.op("dve", lambda e, b=b, Akp=Akp: e.tensor_tensor(out=Akp[0][:, :, :].rearrange("p u s -> p (u s)"),
                                                                in0=cs("amask", 64), in1=PB(b)[0:64, 0:256], op=ALU.mult),
                 [bk(b), "CST"], ["Ak0_%d" % pg])
            P.op("act", lambda e, b=b, Z32=Z32: e.activation(out=Z32[:, :, 64:128],
                                                             in_=PB(b)[0:64, 256:512].rearrange("p (u v) -> p u v", v=64),
                                                             func=AF.Copy), [bk(b)], [z32k])
            P.op("pool", lambda e, c0=c0, Z32=Z32: e.tensor_copy(out=Z32[:, :, 0:64].rearrange("p (c h) k -> p c h k", h=2),
                                                                 in_=TM[:, c0:c0 + 2, 0, :].rearrange("p c (h k) -> p c h k", k=64)),
                 ["TM"], [z32k])
            P.op("pool", lambda e, Nkp=Nkp, Nm=Nm: e.tensor_copy(out=Nkp[0][:, :, :], in_=Nm[:, :, 0, :]), [nmk], ["Nk0_%d" % pg])
            P.op("act", lambda e, Zbf=Zbf, Z32=Z32: e.activation(out=Zbf[:, :, :], in_=Z32[:, :, :], func=AF.Copy), [z32k], [zbk])
        for lvl in range(6):
            for pg in range(4):
                Z32, z32k, Zbf, zbk = Z32L[pg], "Z32%d" % pg, ZbfL[pg], "Zbf%d" % pg
                pi = lvl % 2
                Ak, Nk = AkpL[pg][pi], NkpL[pg][pi]
                akk, nkk = "Ak%d_%d" % (pi, pg), "Nk%d_%d" % (pi, pg)
                b = P.bank()
                for u in range(4):
                    P.op("pe", lambda e, u=u, b=b, Nk=Nk, Zbf=Zbf: e.matmul(PB(b)[0:64, u * 128:(u + 1) * 128], lhsT=Nk[:, u, :],
                                                                            rhs=Zbf[:, u, :], start=True, stop=True),
                         [nkk, zbk], [bk(b)])
                if lvl < 5:
                    b2 = P.bank()
                    for u in range(4):
                        P.op("pe", lambda e, u=u, b2=b2, Nk=Nk, Ak=Ak: e.matmul(
                            PB(b2)[0:64, u * 64:(u + 1) * 64], lhsT=Ak[:, u, :], rhs=Nk[:, u, :], start=True, stop=True),
                             [akk, nkk], [bk(b2)])
                        P.op("pe", lambda e, u=u, b2=b2, Nk=Nk, Ak=Ak: e.matmul(
                            PB(b2)[0:64, 256 + u * 64:256 + (u + 1) * 64], lhsT=Nk[:, u, :], rhs=Ak[:, u, :],
                            start=True, stop=True), [akk, nkk], [bk(b2)])
                P.op("dve", lambda e, b=b, Z32=Z32: e.tensor_tensor(out=Z32[:, :, :].rearrange("p u k -> p (u k)"),
                                                                    in0=Z32[:, :, :].rearrange("p u k -> p (u k)"),
                                                                    in1=PB(b)[0:64, :], op=ALU.add), [bk(b), z32k], [z32k])
                P.op("act", lambda e, Zbf=Zbf, Z32=Z32: e.activation(out=Zbf[:, :, :], in_=Z32[:, :, :], func=AF.Copy), [z32k], [zbk])
                if lvl < 5:
                    P.op("act", lambda e, b2=b2, pi=pi, pg=pg: e.activation(
                        out=NkpL[pg][1 - pi][:, :, :].rearrange("p u s -> p (u s)"), in_=PB(b2)[0:64, 0:256], func=AF.Copy),
                         [bk(b2)], ["Nk%d_%d" % (1 - pi, pg)])
                    P.op("dve", lambda e, b2=b2, pi=pi, pg=pg: e.tensor_copy(
                        out=AkpL[pg][1 - pi][:, :, :].rearrange("p u s -> p (u s)"), in_=PB(b2)[0:64, 256:512]),
                         [bk(b2)], ["Ak%d_%d" % (1 - pi, pg)])
        for pg in range(4):
            c0 = pg * 2
            Nm, nmk = NmL[pg], "Nm%d" % pg
            Zbf, zbk = ZbfL[pg], "Zbf%d" % pg
            RhT, MT = RhTL[pg], MTL[pg]
            b = P.bank()
            for u in range(4):
                cc, h = u // 2, u % 2
                c = c0 + cc
                P.op("pe", lambda e, u=u, cc=cc, h=h, b=b, Zbf=Zbf, Nm=Nm: e.matmul(
                    PB(b)[h * 64:(h + 1) * 64, cc * 64:(cc + 1) * 64], lhsT=Zbf[:, u, 0:64], rhs=Nm[:, u, 1, :],
                    start=True, stop=True), [zbk, nmk], [bk(b)])
                P.op("pe", lambda e, u=u, cc=cc, h=h, c=c, b=b, Zbf=Zbf: e.matmul(
                    PB(b)[h * 64:(h + 1) * 64, 128 + cc * 64:128 + (cc + 1) * 64], lhsT=Zbf[:, u, 0:64],
                    rhs=TM[:, c, 1, h * 64:(h + 1) * 64], start=True, stop=True), [zbk, "TM"], [bk(b)])
            P.op("dve", lambda e, b=b, c0=c0, RhT=RhT: e.tensor_tensor(out=RhT[:, :, :], in0=v3(r_rt32)[:, c0:c0 + 2, :],
                                                                       in1=PB(b)[:, 0:128].rearrange("p (c t) -> p c t", t=64), op=ALU.add),
                 [bk(b), "r_rt32"], ["RhT%d" % pg])
            P.op("act", lambda e, b=b, MT=MT: e.activation(out=MT[:, :, :].rearrange("p c k -> p (c k)"), in_=PB(b)[:, 128:256],
                                                           func=AF.Copy), [bk(b)], ["MT%d" % pg])
        capW = P.end_capture()
        P.replay_merged([capH, capW] + ([EXTRA["cap"]] if EXTRA["cap"] else []))
        EXTRA["cap"] = None
        for pg in range(4):
            c0 = pg * 2
            Nm, nmk = NmL[pg], "Nm%d" % pg
            Zbf, zbk = ZbfL[pg], "Zbf%d" % pg
            RhT, rhk, MT, mtk = RhTL[pg], "RhT%d" % pg, MTL[pg], "MT%d" % pg
            by = P.bank()
            for cc in range(2):
                c = c0 + cc
                si_ = state["sidx"]
                Sb, Sk = Sbf[si_ % 2], "Sbf%d" % (si_ % 2)
                Sb2, Sk2 = Sbf[(si_ + 1) % 2], "Sbf%d" % ((si_ + 1) % 2)
                state["sidx"] += 1
                obk = ob_banks[c // 4]
                oreg = PB(obk)[0:64, (c % 4) * 128:(c % 4 + 1) * 128]
                P.op("pe", lambda e, c=c, oreg=oreg: e.matmul(oreg, lhsT=h_scT[:, c, :], rhs=Vh[:, c, :], start=True, stop=False),
                     ["h_scT", "Vh"], [bk(obk)])
                P.op("pe", lambda e, c=c, oreg=oreg, Sb=Sb: e.matmul(oreg, lhsT=h_qhat[:, c * 64:(c + 1) * 64], rhs=Sb[:, :],
                                                                     start=False, stop=True), ["h_qhat", Sk], [bk(obk)])
                bS = P.bank()
                P.op("pe", lambda e, c=c, bS=bS: e.matmul(PB(bS)[:, 0:128], lhsT=h_khT[:, c, :], rhs=Vh[:, c, :],
                                                          start=True, stop=True), ["h_khT", "Vh"], [bk(bS)])
                P.op("dve", lambda e, c=c, bS=bS: e.scalar_tensor_tensor(out=S32[:, :], in0=S32[:, :], scalar=h_dg[:, c:c + 1],
                                                                         in1=PB(bS)[:, 0:128], op0=ALU.mult, op1=ALU.add),
                     ["S32", "h_dg", bk(bS)], ["S32"])
                P.op("act", lambda e, Sb2=Sb2: e.activation(out=Sb2[:, :], in_=S32[:, :], func=AF.Copy), ["S32"], [Sk2])
                hi_ = state["hidx"]
                Hb, Hk = Hbf[hi_ % 2], "Hbf%d" % (hi_ % 2)
                Hb2, Hk2 = Hbf[(hi_ + 1) % 2], "Hbf%d" % ((hi_ + 1) % 2)
                state["hidx"] += 1
                bH = P.bank()
                for h in range(2):
                    u = cc * 2 + h
                    ph = slice(h * 64, (h + 1) * 64)
                    yreg = PB(by)[0:64, cc * 128 + h * 64:cc * 128 + (h + 1) * 64]
                    P.op("pe", lambda e, u=u, yreg=yreg, Nm=Nm, Zbf=Zbf: e.matmul(yreg, lhsT=Nm[:, u, 1, :], rhs=Zbf[:, u, 64:128],
                                                                                  start=True, stop=False), [nmk, zbk], [bk(by)])
                    P.op("pe", lambda e, u=u, yreg=yreg, c=c, h=h, Nm=Nm: e.matmul(yreg, lhsT=Nm[:, u, 3, :],
                                                                                   rhs=Vr[:, c, h * 64:(h + 1) * 64],
                                                                                   start=False, stop=False), [nmk, kVr], [bk(by)])
                    P.op("pe", lambda e, yreg=yreg, ph=ph, cc=cc, Hb=Hb, RhT=RhT: e.matmul(yreg, lhsT=RhT[ph, cc, :], rhs=Hb[ph, :],
                                                                                           start=False, stop=True),
                         [rhk, Hk], [bk(by)], rg=h * 64)
                    hreg = PB(bH)[ph, 0:64]
                    P.op("pe", lambda e, u=u, hreg=hreg, c=c, h=h, Zbf=Zbf: e.matmul(hreg, lhsT=TM[:, c, 1, h * 64:(h + 1) * 64],
                                                                                     rhs=Zbf[:, u, 64:128], start=True, stop=False),
                         ["TM", zbk], [bk(bH)])
                    P.op("pe", lambda e, hreg=hreg, c=c, h=h: e.matmul(hreg, lhsT=TM[:, c, 2, h * 64:(h + 1) * 64],
                                                                       rhs=Vr[:, c, h * 64:(h + 1) * 64], start=False, stop=False),
                         ["TM", kVr], [bk(bH)])
                    P.op("pe", lambda e, hreg=hreg, ph=ph, cc=cc, Hb=Hb, MT=MT: e.matmul(hreg, lhsT=MT[ph, cc, :], rhs=Hb[ph, :],
                                                                                         start=False, stop=True),
                         [mtk, Hk], [bk(bH)], rg=h * 64)
                P.op("dve", lambda e, c=c, bH=bH: e.scalar_tensor_tensor(out=H32[:, :], in0=H32[:, :], scalar=r_dg[:, c:c + 1],
                                                                         in1=PB(bH)[:, 0:64], op0=ALU.mult, op1=ALU.add),
                     ["H32", "r_dg", bk(bH)], ["H32"])
                P.op("act", lambda e, Hb2=Hb2: e.activation(out=Hb2[:, :], in_=H32[:, :], func=AF.Copy), ["H32"], [Hk2])
            P.op("act", lambda e, by=by, c0=c0: e.activation(out=Yt[:, c0:c0 + 2, :].rearrange("p c k -> p (c k)"),
                                                             in_=PB(by)[0:64, 0:256], func=AF.Copy), [bk(by)], ["Yt"])
        return None


    import os
    KSTOP = os.environ.get("KSTOP", "")
    if KSTOP == "A0":
        P.emit([])
        return nc
    def issue_cc(blk):
        q = ((blk + 1) * 512 - 1) // NT
        if (blk + 1) * 512 % NT == 0 or NT < 512:
            qs = [q] if NT >= 512 else list(range(4))
            for qq in qs:
                if os.environ.get("KSKIP_CC"):
                    P.dma("pool", xout[qq][:, :], xin[qq][:, :], "ccfake", reads=["xin%d" % qq], writes=["xout%d" % qq])
                    continue
                P.op("pool", lambda e, qq=qq: e.collective_compute(
                    "AllReduce", ALU.add, replica_groups=[[0, 1, 2, 3], [4, 5, 6, 7]],
                    ins=[xin[qq].ap().opt()], outs=[xout[qq].ap().opt()]),
                     reads=["xin%d" % qq], writes=["xout%d" % qq], sem="CC", inc=1)

    try:
        P.begin_capture([0, 1, 2, 3])
        phaseA_block(0, "norm")
        phaseA_block(0, "front2")
        P.replay_merged([P.end_capture()])
        for blk in range(NB):
            if blk + 1 < NB:
                P.begin_capture([5])
                phaseA_block(blk + 1, "norm")
                EXTRA["cap"] = P.end_capture()
            phaseA_block(blk, "rest")
            P.begin_capture([4, 5])
            phaseA_out(blk)
            caps = [P.end_capture()]
            if blk + 1 < NB:
                P.begin_capture([0, 1, 2, 3])
                phaseA_block(blk + 1, "front2")
                caps.append(P.end_capture())
            P.replay_merged(caps)
            chk("A7")
            issue_cc(blk)
    except _Stop:
        P.emit([])
        return nc
    if KSTOP == "A":
        P.emit([])
        return nc
    P.barrier()
    stA.close()
    P.nb = 8

    stB = contextlib.ExitStack()
    cur[0] = stP
    h2T = sb([128, 8, NT], BF16)
    Wt = sb([128, NTT, 32])
    gt1b = sb([128, D])
    gt2b = sb([128, D])
    fgb_t = sb([128, D])
    cur[0] = stB
    fgb = din("fgb", [128, D])
    xown = din("xown", [NT, D])
    P.dma("sp", fgb_t[:, :], fgb[:, :], "fgb", writes=["fgb"])
    dgt = [sb([128, 128]) for _ in range(2)]
    di = [0]

    def row_bcast(vec, out_t, outk):
        for half in range(2):
            b = P.bank()
            for k4 in range(4):
                kc = half * 4 + k4
                d_, dk = dgt[di[0] % 2], "dgt%d" % (di[0] % 2)
                di[0] += 1
                P.op("dve", lambda e, d_=d_, kc=kc: e.tensor_scalar(out=d_[:, :], in0=cs("ident"), scalar1=vec[:, kc:kc + 1],
                                                                    scalar2=None, op0=ALU.mult), ["CST", "mod"], [dk])
                P.op("pe", lambda e, d_=d_, k4=k4, b=b: e.matmul(PB(b)[:, k4 * 128:(k4 + 1) * 128], lhsT=cs("ones"), rhs=d_[:, :],
                                                                 start=True, stop=True), ["CST", dk], [bk(b)])
            P.op("act", lambda e, half=half, b=b: e.activation(out=out_t[:, half * 512:(half + 1) * 512], in_=PB(b)[:, :],
                                                               func=AF.Copy), [bk(b)], [outk])

    row_bcast(mod[:, 16:24], gt1b, "gt1b")
    row_bcast(mod[:, 40:48], gt2b, "gt2b")

    NF["junk"] = sb([128, D], BF16)
    NF["xn"] = [sb([128, D], BF16) for _ in range(2)]
    NF["stat"] = [sb([128, 4]) for _ in range(2)]
    junk, stat = NF["junk"], NF["stat"]
    stgB = [sb([128, 8, 256]) for _ in range(2)]
    sbi = [0]

    def load_castB(dst_fn, src_ap, nk, ncols, wk):
        t, k = stgB[sbi[0] % 2], "stgB%d" % (sbi[0] % 2)
        eng = "sp" if sbi[0] % 2 == 0 else "act"
        sbi[0] += 1
        P.dma(eng, t[:, 0:nk, 0:ncols], src_ap.rearrange("(kc p) n -> p kc n", p=128), k, writes=[k])
        for kc in range(nk):
            if kc % 2:
                P.op("act", lambda e, kc=kc: e.activation(out=dst_fn(kc), in_=t[:, kc, 0:ncols], func=AF.Copy), [k], [wk])
            else:
                P.op("dve", lambda e, kc=kc: e.tensor_copy(out=dst_fn(kc), in_=t[:, kc, 0:ncols]), [k], [wk])

    Wgt = sb([128, 8, 2048], BF16)
    Wpa = sb([128, 4, D], BF16)
    Wpb = sb([128, 4, D], BF16)
    Wo = sb([128, 8, D], BF16)
    Wr32 = sb([128, 8, 36])
    for c4 in range(8):
        load_castB(lambda kc, c4=c4: Wgt[:, kc, c4 * 256:(c4 + 1) * 256], wgates[:, c4 * 256:(c4 + 1) * 256], 8, 256, "WB")
    for c2 in range(4):
        load_castB(lambda kc, c2=c2: Wpa[:, kc, c2 * 256:(c2 + 1) * 256], wpa[:, c2 * 256:(c2 + 1) * 256], 4, 256, "WB")
        load_castB(lambda kc, c2=c2: Wpb[:, kc, c2 * 256:(c2 + 1) * 256], wpb[:, c2 * 256:(c2 + 1) * 256], 4, 256, "WB")
        load_castB(lambda kc, c2=c2: Wo[:, kc, c2 * 256:(c2 + 1) * 256], wout[:, c2 * 256:(c2 + 1) * 256], 8, 256, "WB")
    P.dma("sp", Wr32[:, :, :], wr[:, :].rearrange("(kc p) n -> p kc n", p=128), "wr", writes=["WB"])

    TB = min(256, NT)
    TBT = TB // 128
    xB = sb([128, TBT, D])
    hB = sb([128, 8, TB], BF16)
    oT = sb([128, 8, TB], BF16)
    mixT = sb([128, 8, TB], BF16)
    xo = [sb([128, D], BF16) for _ in range(4)]
    oc = sb([128, D], BF16)
    gsa = sb([128, TB])
    gsb = sb([128, TB])
    m1t = sb([128, TB])
    m2t = sb([128, TB])
    x1t = [sb([128, D]) for _ in range(2)]
    x1n = sb([128, D])
    h32 = sb([128, 8, 128])
    rt_ = sb([128, 64])
    L = sb([128, 36])
    lem = sb([128, 32])
    lem2 = sb([128, 32])
    mk1 = sb([128, 32])
    mk2 = sb([128, 32])
    identf = cs("ident")

    for blk in range(NT // TB):
        for tt in range(TBT):
            r0 = blk * TB + tt * 128
            P.dma("sp", xB[:, tt, :], xown[r0:r0 + 128, :], "xB", writes=["xB"])
            i = nti[0] % 2
            norm_fm(xB[:, tt, :], "xB", s1, mod, "s1", lambda kc, tt=tt: hB[:, kc, tt * 128:(tt + 1) * 128], "hB")
            for qq in range(4):
                P.dma("act" if qq % 2 else "sp", xo[qq][:, :], xout[qq][r0:r0 + 128, :], "xo%d" % qq,
                      reads=["xout%d" % qq], writes=["xo%d" % qq])
            P.op("dve", lambda e: e.tensor_scalar(out=oc[:, :], in0=xo[0][:, :], scalar1=pvs("sel")[:, 0:1], scalar2=None,
                                                  op0=ALU.mult), ["xo0", "PV"], ["oc"])
            for qq in range(1, 4):
                P.op("dve", lambda e, qq=qq: e.scalar_tensor_tensor(out=oc[:, :], in0=xo[qq][:, :], scalar=pvs("sel")[:, qq:qq + 1],
                                                                    in1=oc[:, :], op0=ALU.mult, op1=ALU.add),
                     ["xo%d" % qq, "oc", "PV"], ["oc"])
            b = P.bank()
            for ch in range(8):
                P.op("pe", lambda e, ch=ch, b=b: e.transpose(out=PBbf(b)[:, ch * 128:(ch + 1) * 128],
                                                             in_=oc[:, ch * 128:(ch + 1) * 128], identity=identb[:, :]),
                     ["oc", "identb"], [bk(b)])
            P.op("act", lambda e, tt=tt, b=b: e.activation(out=oT[:, :, tt * 128:(tt + 1) * 128],
                                                           in_=PBbf(b)[:, :].rearrange("p (c t) -> p c t", t=128), func=AF.Copy),
                 [bk(b)], ["oT"])
        for mc in range(8):
            ba, bb_, bga, bgb = P.bank(), P.bank(), P.bank(), P.bank()
            for j_ in range(4):
                P.op("pe", lambda e, j_=j_, mc=mc, ba=ba: e.matmul(PB(ba)[:, 0:TB], lhsT=Wpa[:, j_, mc * 128:(mc + 1) * 128],
                                                                   rhs=oT[:, j_ * 2, :], start=(j_ == 0), stop=(j_ == 3)),
                     ["WB", "oT"], [bk(ba)])
            for j_ in range(4):
                P.op("pe", lambda e, j_=j_, mc=mc, bb_=bb_: e.matmul(PB(bb_)[:, 0:TB], lhsT=Wpb[:, j_, mc * 128:(mc + 1) * 128],
                                                                     rhs=oT[:, j_ * 2 + 1, :], start=(j_ == 0), stop=(j_ == 3)),
                     ["WB", "oT"], [bk(bb_)])
            for kc in range(8):
                P.op("pe", lambda e, kc=kc, mc=mc, bga=bga: e.matmul(PB(bga)[:, 0:TB], lhsT=Wgt[:, kc, mc * 128:(mc + 1) * 128],
                                                                     rhs=hB[:, kc, :], start=(kc == 0), stop=(kc == 7)),
                     ["WB", "hB"], [bk(bga)])
            for kc in range(8):
                P.op("pe", lambda e, kc=kc, mc=mc, bgb=bgb: e.matmul(PB(bgb)[:, 0:TB], lhsT=Wgt[:, kc, D + mc * 128:D + (mc + 1) * 128],
                                                                     rhs=hB[:, kc, :], start=(kc == 0), stop=(kc == 7)),
                     ["WB", "hB"], [bk(bgb)])
            P.op("act", lambda e, bga=bga: e.activation(out=gsa[:, :], in_=PB(bga)[:, 0:TB], func=AF.Sigmoid), [bk(bga)], ["gsa"])
            P.op("act", lambda e, bgb=bgb: e.activation(out=gsb[:, :], in_=PB(bgb)[:, 0:TB], func=AF.Sigmoid), [bk(bgb)], ["gsb"])
            P.op("dve", lambda e, ba=ba: e.tensor_tensor(out=m1t[:, :], in0=gsa[:, :], in1=PB(ba)[:, 0:TB], op=ALU.mult),
                 ["gsa", bk(ba)], ["m1t"])
            P.op("dve", lambda e, bb_=bb_: e.tensor_tensor(out=m2t[:, :], in0=gsb[:, :], in1=PB(bb_)[:, 0:TB], op=ALU.mult),
                 ["gsb", bk(bb_)], ["m2t"])
            P.op("pool", lambda e, mc=mc: e.tensor_tensor(out=mixT[:, mc, :], in0=m1t[:, :], in1=m2t[:, :], op=ALU.add),
                 ["m1t", "m2t"], ["mixT"])
        for tt in range(TBT):
            tile_i = blk * TBT + tt
            r0 = tile_i * 128
            x1, x1k = x1t[tile_i % 2], "x1t%d" % (tile_i % 2)
            for ch in range(2):
                b = P.bank()
                for kc in range(8):
                    P.op("pe", lambda e, kc=kc, tt=tt, ch=ch, b=b: e.matmul(PB(b)[:, :], lhsT=mixT[:, kc, tt * 128:(tt + 1) * 128],
                                                                            rhs=Wo[:, kc, ch * 512:(ch + 1) * 512],
                                                                            start=(kc == 0), stop=(kc == 7)), ["mixT", "WB"], [bk(b)])
                P.op("dve", lambda e, ch=ch, b=b, x1=x1: e.tensor_tensor(out=x1[:, ch * 512:(ch + 1) * 512], in0=PB(b)[:, :],
                                                                         in1=gt1b[:, ch * 512:(ch + 1) * 512], op=ALU.mult),
                     [bk(b), "gt1b"], [x1k])
            P.op("pool", lambda e, tt=tt, x1=x1: e.tensor_tensor(out=x1[:, :], in0=x1[:, :], in1=xB[:, tt, :], op=ALU.add),
                 [x1k, "xB"], [x1k])
            P.dma("act", x1s[r0:r0 + 128, :], x1[:, :], "x1st%d" % (tile_i % 2), reads=[x1k], writes=["x1s"])
            st_, sk = stat[tile_i % 2], "stat%d" % (tile_i % 2)
            P.op("act", lambda e, x1=x1, st_=st_: e.activation(out=junk[:, :], in_=x1[:, :], func=AF.Square, accum_out=st_[:, 0:1]),
                 [x1k], ["junk", sk])
            P.op("dve", lambda e, st_=st_: e.tensor_scalar(out=st_[:, 1:2], in0=st_[:, 0:1], scalar1=1.0 / D, scalar2=1e-6,
                                                           op0=ALU.mult, op1=ALU.add), [sk], [sk])
            rsq(st_[:, 2:3], st_[:, 1:2], [sk], [sk])
            P.op("dve", lambda e, x1=x1, st_=st_: e.tensor_scalar(out=x1n[:, :], in0=x1[:, :], scalar1=st_[:, 2:3], scalar2=None,
                                                                  op0=ALU.mult), [x1k, sk], ["x1n"])
            for half in range(2):
                b = P.bank()
                for k4 in range(4):
                    kc = half * 4 + k4
                    P.op("pe", lambda e, kc=kc, k4=k4, b=b: e.transpose(out=PB(b)[:, k4 * 128:(k4 + 1) * 128],
                                                                        in_=x1n[:, kc * 128:(kc + 1) * 128], identity=identf),
                         ["x1n", "CST"], [bk(b)])
                for k4 in range(4):
                    kc = half * 4 + k4
                    P.op("act", lambda e, kc=kc, k4=k4, b=b: e.activation(out=h32[:, kc, :], in_=PB(b)[:, k4 * 128:(k4 + 1) * 128],
                                                                          func=AF.Identity, scale=s2[:, kc:kc + 1],
                                                                          bias=mod[:, 24 + kc:25 + kc]), [bk(b), "s2", "mod"], ["h32"])
            P.op("pool", lambda e, r0=r0: e.tensor_copy(out=h2T[:, :, r0:r0 + 128], in_=h32[:, :, :]), ["h32"], ["h2T"])
            b = P.bank()
            for kc in range(8):
                P.op("pe", lambda e, kc=kc, b=b: e.matmul(PB(b)[:, 0:36], lhsT=h32[:, kc, :], rhs=Wr32[:, kc, :],
                                                          start=(kc == 0), stop=(kc == 7)), ["h32", "WB"], [bk(b)])
            R = lambda a, b_: rt_[:, a:b_]
            V = lambda fn, rd, wr_: P.op("dve", fn, rd, wr_)
            V(lambda e, b=b: e.tensor_tensor(out=L[:, :], in0=PB(b)[:, 0:36], in1=pvs("rb"), op=ALU.add), [bk(b), "PV"], ["L"])
            V(lambda e: e.tensor_reduce(out=R(0, 1), in_=L[:, 0:4], axis=AX.X, op=ALU.max), ["L"], ["rt"])
            V(lambda e: e.tensor_scalar(out=R(1, 2), in0=R(0, 1), scalar1=-1.0, scalar2=None, op0=ALU.mult), ["rt"], ["rt"])
            P.op("act", lambda e: e.activation(out=R(4, 8), in_=L[:, 0:4], func=AF.Exp, bias=R(1, 2), accum_out=R(2, 3)),
                 ["L", "rt"], ["rt"])
            V(lambda e: e.reciprocal(out=R(3, 4), in_=R(2, 3)), ["rt"], ["rt"])
            V(lambda e: e.tensor_scalar(out=R(8, 12), in0=L[:, 0:4], scalar1=R(0, 1), scalar2=None, op0=ALU.is_equal),
              ["L", "rt"], ["rt"])
            V(lambda e: e.tensor_scalar(out=R(12, 16), in0=R(8, 12), scalar1=-1.0, scalar2=1e30, op0=ALU.add, op1=ALU.mult),
              ["rt"], ["rt"])
            V(lambda e: e.tensor_tensor(out=lem[:, :].rearrange("p (g k) -> p g k", k=8),
                                        in0=L[:, 4:36].rearrange("p (g k) -> p g k", k=8),
                                        in1=R(12, 16).unsqueeze(2).to_broadcast([128, 4, 8]), op=ALU.add), ["L", "rt"], ["lem"])
            V(lambda e: e.tensor_reduce(out=R(16, 17), in_=lem[:, :], axis=AX.X, op=ALU.max), ["lem"], ["rt"])
            V(lambda e: e.tensor_scalar(out=mk1[:, :], in0=lem[:, :], scalar1=R(16, 17), scalar2=None, op0=ALU.is_equal),
              ["lem", "rt"], ["mk1"])
            V(lambda e: e.scalar_tensor_tensor(out=lem2[:, :], in0=mk1[:, :], scalar=-1e30, in1=lem[:, :], op0=ALU.mult, op1=ALU.add),
              ["mk1", "lem"], ["lem2"])
            V(lambda e: e.tensor_reduce(out=R(17, 18), in_=lem2[:, :], axis=AX.X, op=ALU.max), ["lem2"], ["rt"])
            V(lambda e: e.tensor_scalar(out=mk2[:, :], in0=lem2[:, :], scalar1=R(17, 18), scalar2=None, op0=ALU.is_equal),
              ["lem2", "rt"], ["mk2"])
            V(lambda e: e.tensor_scalar(out=R(18, 19), in0=R(16, 17), scalar1=-1.0, scalar2=None, op0=ALU.mult), ["rt"], ["rt"])
            P.op("act", lambda e: e.activation(out=R(19, 20), in_=R(17, 18), func=AF.Exp, bias=R(18, 19)), ["rt"], ["rt"])
            V(lambda e: e.tensor_scalar(out=R(20, 21), in0=R(19, 20), scalar1=1.0, scalar2=None, op0=ALU.add), ["rt"], ["rt"])
            V(lambda e: e.reciprocal(out=R(21, 22), in_=R(20, 21)), ["rt"], ["rt"])
            V(lambda e: e.tensor_tensor(out=R(22, 23), in0=R(21, 22), in1=R(3, 4), op=ALU.mult), ["rt"], ["rt"])
            V(lambda e: e.tensor_tensor(out=R(23, 24), in0=R(22, 23), in1=R(19, 20), op=ALU.mult), ["rt"], ["rt"])
            V(lambda e: e.tensor_scalar(out=mk1[:, :], in0=mk1[:, :], scalar1=R(22, 23), scalar2=None, op0=ALU.mult),
              ["mk1", "rt"], ["mk1"])
            V(lambda e, tile_i=tile_i: e.scalar_tensor_tensor(out=Wt[:, tile_i, :], in0=mk2[:, :], scalar=R(23, 24), in1=mk1[:, :],
                                                              op0=ALU.mult, op1=ALU.add), ["mk2", "mk1", "rt"], ["Wt"])
    P.barrier()
    stB.close()

    stM = contextlib.ExitStack()
    cur[0] = stM
    TGT = TG // 128
    acc = sb([128, TGT, D])
    stgM = [sb([128, 8, 256]) for _ in range(2)]
    wG = [sb([128, 8, 512], BF16) for _ in range(2)]
    wU = [sb([128, 8, 512], BF16) for _ in range(2)]
    wD = [sb([128, 4, D], BF16) for _ in range(2)]
    sgl = [sb([128, 512]) for _ in range(2)]
    hid = [sb([128, 4, 512], BF16) for _ in range(2)]
    x1r = [sb([128, D]) for _ in range(2)]
    x2 = [sb([128, D]) for _ in range(2)]
    yo = [sb([128, D]) for _ in range(2)]
    junk2 = sb([128, D], BF16)
    stat2 = [sb([128, 4]) for _ in range(2)]
    mi = [0]
    TBm = min(512, TG)

    def load_castM(dst, dk, src_ap, nk, ncols_total):
        for c0_ in range(0, ncols_total, 256):
            t, k = stgM[mi[0] % 2], "stgM%d" % (mi[0] % 2)
            eng = "sp" if mi[0] % 2 == 0 else "act"
            mi[0] += 1
            P.dma(eng, t[:, 0:nk, :], src_ap[:, c0_:c0_ + 256].rearrange("(kc p) n -> p kc n", p=128), k, writes=[k])
            P.op("act", lambda e, t=t, c0_=c0_: e.activation(out=dst[:, 0:nk // 2, c0_:c0_ + 256], in_=t[:, 0:nk // 2, :], func=AF.Copy),
                 [k], [dk])
            P.op("pool", lambda e, t=t, c0_=c0_: e.tensor_copy(out=dst[:, nk // 2:nk, c0_:c0_ + 256], in_=t[:, nk // 2:nk, :]), [k], [dk])

    hcount = [0]
    for gi in range(NG):
        g0 = gi * TG
        P.op("pool", lambda e: e.memset(acc[:, :, :], 0.0), [], ["acc"])
        def emit_G(ex, bl):
            wi = ex % 2
            t0 = g0 + bl * TBm
            hd, hdk = hid[hcount[0] % 2], "hid%d" % (hcount[0] % 2)
            hcount[0] += 1
            for fc in range(4):
                bg, bu = P.bank(), P.bank()
                for kc in range(8):
                    P.op("pe", lambda e, kc=kc, fc=fc, bg=bg, wi=wi, t0=t0: e.matmul(
                        PB(bg)[:, 0:TBm], lhsT=wG[wi][:, kc, fc * 128:(fc + 1) * 128], rhs=h2T[:, kc, t0:t0 + TBm],
                        start=(kc == 0), stop=(kc == 7)), ["wG%d" % wi, "h2T"], [bk(bg)])
                for kc in range(8):
                    P.op("pe", lambda e, kc=kc, fc=fc, bu=bu, wi=wi, t0=t0: e.matmul(
                        PB(bu)[:, 0:TBm], lhsT=wU[wi][:, kc, fc * 128:(fc + 1) * 128], rhs=h2T[:, kc, t0:t0 + TBm],
                        start=(kc == 0), stop=(kc == 7)), ["wU%d" % wi, "h2T"], [bk(bu)])
                sg_, sgk = sgl[fc % 2], "sgl%d" % (fc % 2)
                P.op("act", lambda e, bg=bg, sg_=sg_: e.activation(out=sg_[:, 0:TBm], in_=PB(bg)[:, 0:TBm], func=AF.Silu),
                     [bk(bg)], [sgk])
                P.op("dve", lambda e, bu=bu, sg_=sg_, hd=hd, fc=fc: e.tensor_tensor(out=hd[:, fc, 0:TBm], in0=sg_[:, 0:TBm],
                                                                                    in1=PB(bu)[:, 0:TBm], op=ALU.mult),
                     [sgk, bk(bu)], [hdk])
            return hd, hdk

        def emit_D(ex, bl, hd, hdk):
            wi = ex % 2
            for tt in range(TBm // 128):
                lt_ = bl * (TBm // 128) + tt
                gt_ = gi * TGT + lt_
                for ch in range(2):
                    b = P.bank()
                    for fc in range(4):
                        P.op("pe", lambda e, fc=fc, tt=tt, ch=ch, b=b, hd=hd, wi=wi: e.matmul(
                            PB(b)[:, :], lhsT=hd[:, fc, tt * 128:(tt + 1) * 128], rhs=wD[wi][:, fc, ch * 512:(ch + 1) * 512],
                            start=(fc == 0), stop=(fc == 3)), [hdk, "wD%d" % wi], [bk(b)])
                    P.op("dve", lambda e, b=b, lt_=lt_, gt_=gt_, ch=ch, ex=ex: e.scalar_tensor_tensor(
                        out=acc[:, lt_, ch * 512:(ch + 1) * 512], in0=PB(b)[:, :], scalar=Wt[:, gt_, ex:ex + 1],
                        in1=acc[:, lt_, ch * 512:(ch + 1) * 512], op0=ALU.mult, op1=ALU.add), [bk(b), "Wt", "acc"], ["acc"])

        prev = None
        for ex in range(32):
            wi = ex % 2
            load_castM(wG[wi], "wG%d" % wi, eg[ex], 8, 512)
            load_castM(wU[wi], "wU%d" % wi, eu[ex], 8, 512)
            load_castM(wD[wi], "wD%d" % wi, ed[ex], 4, D)
            for bl in range(TG // TBm):
                cur = emit_G(ex, bl)
                if prev is not None:
                    emit_D(*prev)
                prev = (ex, bl) + cur
        emit_D(*prev)
        for lt_ in range(TGT):
            gt_ = gi * TGT + lt_
            r0 = gt_ * 128
            i = gt_ % 2
            P.dma("sp", x1r[i][:, :], x1s[r0:r0 + 128, :], "x1r%d" % i, reads=["x1s"], writes=["x1r%d" % i])
            P.op("pool", lambda e, i=i, lt_=lt_: e.tensor_tensor(out=x2[i][:, :], in0=acc[:, lt_, :], in1=gt2b[:, :], op=ALU.mult),
                 ["acc", "gt2b"], ["x2%d" % i])
            P.op("dve", lambda e, i=i: e.tensor_tensor(out=x2[i][:, :], in0=x2[i][:, :], in1=x1r[i][:, :], op=ALU.add),
                 ["x2%d" % i, "x1r%d" % i], ["x2%d" % i])
            P.op("act", lambda e, i=i: e.activation(out=junk2[:, :], in_=x2[i][:, :], func=AF.Square, accum_out=stat2[i][:, 0:1]),
                 ["x2%d" % i], ["junk2", "st2%d" % i])
            P.op("dve", lambda e, i=i: e.tensor_scalar(out=stat2[i][:, 1:2], in0=stat2[i][:, 0:1], scalar1=1.0 / D, scalar2=1e-6,
                                                       op0=ALU.mult, op1=ALU.add), ["st2%d" % i], ["st2%d" % i])
            rsq(stat2[i][:, 2:3], stat2[i][:, 1:2], ["st2%d" % i], ["st2%d" % i])
            P.op("dve", lambda e, i=i: e.scalar_tensor_tensor(out=yo[i][:, :], in0=x2[i][:, :], scalar=stat2[i][:, 2:3],
                                                              in1=fgb_t[:, :], op0=ALU.mult, op1=ALU.mult),
                 ["x2%d" % i, "st2%d" % i, "fgb"], ["yo%d" % i])
            P.dma("sp", y[r0:r0 + 128, :], yo[i][:, :], "yst%d" % i, reads=["yo%d" % i], writes=["y"])
    finals = [(k, v) for k, v in P.cnt.items() if k.startswith("D:yst")]
    P.emit(finals)
    stM.close()
    stP.close()
    return nc


def _core_inputs(S, b, j, inp, cst_np):
    import os
    NEH = 1 if os.environ.get("KSTOP") else 32
    f = lambda a: np.ascontiguousarray(a, dtype=np.float32)
    NT = S // 4
    w_in = inp["w_in"][0]
    hq, hf, hi_, hg = 0, 512, 1024, 1536
    rw = 2048
    cj = slice(128 * j, 128 * j + 128)
    rws = lambda o: slice(rw + o + 128 * j, rw + o + 128 * j + 128)
    wfm = np.concatenate([w_in[:, hq + 128 * j:hq + 128 * j + 128], w_in[:, hf + 128 * j:hf + 128 * j + 128],
                          w_in[:, rws(0)], w_in[:, rws(512)], w_in[:, rw + 1536:rw + 1696]], 1)
    wtm = np.concatenate([w_in[:, hi_ + 128 * j:hi_ + 128 * j + 128], w_in[:, hg + 128 * j:hg + 128 * j + 128],
                          w_in[:, rws(1024)]], 1)
    mu = inp["rw_mu"][0]
    mu_fm = np.concatenate([mu[0 + 128 * j:128 * j + 128], mu[512 + 128 * j:512 + 128 * j + 128], mu[1536:1696]])
    mu_tm = mu[1024 + 128 * j:1024 + 128 * j + 128]
    col = lambda v: np.asarray(v, np.float32).reshape(128, 1)
    fmv = lambda v: np.asarray(v, np.float32).reshape(-1, 128).T
    bc = lambda v: np.broadcast_to(np.asarray(v, np.float32)[None, :], (128, len(v)))
    sel = np.zeros(4, np.float32)
    sel[j] = 1
    parts = {
        "c": fmv(inp["c"][b]), "ada_b": fmv(inp["ada_b"][0]), "n1g": fmv(inp["norm1_g"][0]), "n2g": fmv(inp["norm2_g"][0]),
        "lb0": col(inp["hg_lb"][0, cj]), "lb1": col(inp["hg_lb"][1, cj]),
        "w0": col(inp["rw_w0"][0, cj]), "a0": col(inp["rw_a0"][0, cj]), "kk": col(inp["rw_kk"][0, cj]),
        "ka": col(inp["rw_ka"][0, cj]), "rk": col(inp["rw_rk"][0, 2 * j:2 * j + 2, :].reshape(128)),
        "mu_fm": bc(mu_fm), "mu_tm": bc(mu_tm), "hgn": bc(inp["hg_norm_g"][0, cj]),
        "gnw": bc(inp["rw_gn_w"][0, cj]), "gnb": bc(inp["rw_gn_b"][0, cj]),
        "rb": bc(np.concatenate([inp["router_g_b"][0], inp["router_e_b"][0]])), "sel": bc(sel),
    }
    pv = np.concatenate([parts[k] for k, _ in PV_LAYOUT], 1)
    lora = np.zeros((128, 384), np.float32)
    lora[0:32, 0:128] = inp["rw_w2"][0][:, cj]
    lora[0:32, 128:256] = inp["rw_a2"][0][:, cj]
    lora[0:96, 256:384] = inp["rw_g2"][0][:, cj]
    xb = inp["x"][b][:S]
    return {
        "x": f(xb), "cst": cst_np, "pv": f(pv), "ada_w": f(inp["ada_w"][0]), "wfm": f(wfm), "wtm": f(wtm), "lora": lora,
        "wgates": f(w_in[:, 3744:5792]), "wpa": f(inp["w_proj_a"][0]), "wpb": f(inp["w_proj_b"][0]), "wout": f(inp["w_out"][0]),
        "wr": f(np.concatenate([inp["router_g_w"][0], inp["router_e_w"][0]], 1)),
        "eg": f(inp["exp_w_gate"][0][:NEH]), "eu": f(inp["exp_w_up"][0][:NEH]), "ed": f(inp["exp_w_down"][0][:NEH]),
        "fgb": f(bc(inp["final_g"])), "xown": f(xb[j * NT:(j + 1) * NT]),
    }


_NC_CACHE = {}


def run(inp, S, trace=False):
    if S not in _NC_CACHE:
        _NC_CACHE[S] = build(S)
    nc = _NC_CACHE[S]
    cst_np, _ = _consts()
    inp = {k: np.asarray(v) for k, v in inp.items()}
    shared = {}
    in_maps = []
    for core in range(8):
        b, j = core // 4, core % 4
        m = _core_inputs(S, b, j, inp, cst_np)
        for k in ("ada_w", "eg", "eu", "ed", "wgates", "wpa", "wpb", "wout", "wr", "fgb", "cst"):
            m[k] = shared.setdefault(k, m[k])
        in_maps.append(m)
    if trace:
        res = run_bass_kernel_spmd(nc, in_maps, core_ids=list(range(8)), trace=True)
        print("EXEC_NS", res.exec_time_ns)
    else:
        res = run_bass_kernel_spmd(nc, in_maps, core_ids=list(range(8)))
    NT = S // 4
    out = np.zeros((2, S, D), np.float32)
    for core in range(8):
        b, j = core // 4, core % 4
        out[b, j * NT:(j + 1) * NT] = res.results[core]["y"]
    return out


def kernel(**inputs):
    return run(inputs, 8192)
```
